# Optimizing a Trainium2 kernel written in Bass

```python
import jax
import jax.numpy as jnp
from jax import lax
import numpy as np

D_MODEL = 1024
BATCH = 8
SEQ = 4096
DEPTH = 2

HEAD_DIM = 64
BRANCH_WIDTH = 256
N_BRANCH = 4
HG_HEADS = 4
HG_CHUNK = 16
LB_FLOOR = 1e-30
RET_HEADS = 4
RET_CHUNK = 64
ATT_HEADS = 4
ATT_KV_HEADS = 2
ATT_GROUP = ATT_HEADS // ATT_KV_HEADS
WINDOW = 128
ATT_BLOCK = 128
MASK_VALUE = -1e30
LRU_WIDTH = 256
LRU_BLOCKS = 4
LRU_BLOCK_DIM = LRU_WIDTH // LRU_BLOCKS
CONV_WIDTH = 4
LRU_C = 8.0
D_FF = 2816
EPS = 1e-6

IN_SIZES = (
    BRANCH_WIDTH, BRANCH_WIDTH, BRANCH_WIDTH, BRANCH_WIDTH,
    BRANCH_WIDTH, BRANCH_WIDTH, BRANCH_WIDTH, BRANCH_WIDTH,
    ATT_HEADS * HEAD_DIM, ATT_KV_HEADS * HEAD_DIM, ATT_KV_HEADS * HEAD_DIM,
    LRU_WIDTH, LRU_WIDTH,
    N_BRANCH * D_MODEL,
)
D_IN = sum(IN_SIZES)

kernel_name = "hybrid_hgrn2_retnet_swa_rglru_macaron"


def _rmsnorm(x, w):
    xf = x.astype(jnp.float32)
    y = xf * lax.rsqrt(jnp.mean(xf * xf, axis=-1, keepdims=True) + EPS)
    return (y * w.astype(jnp.float32)).astype(x.dtype)


def _swiglu(x, wg, wu, wd):
    return (jax.nn.silu(x @ wg) * (x @ wu)) @ wd


def _split_cols(t, sizes):
    out, start = [], 0
    for s in sizes:
        out.append(t[..., start:start + s])
        start += s
    return out


def _to_chunks(t, n_heads, chunk):
    B, T, _ = t.shape
    return t.reshape(B, T // chunk, chunk, n_heads, -1).transpose(0, 3, 1, 2, 4)


def _from_chunks(o):
    B, H, N, C, d = o.shape
    return o.transpose(0, 2, 3, 1, 4).reshape(B, N * C, H * d)


def _chunk_state_scan(decay, dS):
    def step(S, inp):
        dec, ds = inp
        return dec[..., None] * S + ds, S
    S0 = jnp.zeros_like(dS[:, :, 0])
    _, S_start = lax.scan(step, S0, (jnp.moveaxis(decay, 2, 0), jnp.moveaxis(dS, 2, 0)))
    return jnp.moveaxis(S_start, 0, 2)


def _hgrn2(q, f_pre, i, g, lb, out_w):
    B, T, _ = q.shape
    H, d, C = HG_HEADS, HEAD_DIM, HG_CHUNK
    dtype = q.dtype
    z = f_pre.astype(jnp.float32)
    lb = lb.astype(jnp.float32)
    log_f = jnp.logaddexp(jnp.log(jnp.maximum(lb, LB_FLOOR)), jnp.log1p(-lb) + jax.nn.log_sigmoid(z))
    k = (1.0 - lb) * jax.nn.sigmoid(-z)
    qc = _to_chunks(q.astype(jnp.float32), H, C)
    kc = _to_chunks(k, H, C)
    vc = _to_chunks(i.astype(jnp.float32), H, C)
    b = jnp.cumsum(_to_chunks(log_f, H, C), axis=3)
    b_ref = b[:, :, :, C // 2:C // 2 + 1]
    qt = qc * jnp.exp(b - b_ref)
    kt = kc * jnp.exp(b_ref - b)
    causal = jnp.tril(jnp.ones((C, C), dtype=bool))
    att = jnp.where(causal, jnp.einsum('bhntk,bhnsk->bhnts', qt, kt), 0.0)
    o = jnp.einsum('bhnts,bhnsv->bhntv', att, vc)
    b_last = b[:, :, :, -1:]
    dS = jnp.einsum('bhnsk,bhnsv->bhnkv', kc * jnp.exp(b_last - b), vc)
    S_start = _chunk_state_scan(jnp.exp(b_last[:, :, :, 0]), dS)
    o = o + jnp.einsum('bhntk,bhnkv->bhntv', qc * jnp.exp(b), S_start)
    o = _from_chunks(o).reshape(B, T, H, d)
    o = _rmsnorm(o, out_w.reshape(H, d)) * jax.nn.sigmoid(g.astype(jnp.float32)).reshape(B, T, H, d)
    return o.reshape(B, T, H * d).astype(dtype)


def _retention(q, k, v, g, gn_w, gn_b):
    B, T, _ = q.shape
    H, d, C = RET_HEADS, HEAD_DIM, RET_CHUNK
    N = T // C
    dtype = q.dtype
    log_gamma = jnp.log1p(-jnp.exp2(-5.0 - jnp.arange(H, dtype=jnp.float32)))
    qc = _to_chunks(q.astype(jnp.float32), H, C) * d ** -0.5
    kc = _to_chunks(k.astype(jnp.float32), H, C)
    vc = _to_chunks(v.astype(jnp.float32), H, C)
    pos = jnp.arange(C, dtype=jnp.float32)
    rel = pos[:, None] - pos[None, :]
    decay = jnp.where(rel >= 0, jnp.exp(log_gamma[:, None, None] * jnp.maximum(rel, 0.0)), 0.0)
    att = jnp.einsum('bhntk,bhnsk->bhnts', qc, kc) * decay[None, :, None]
    o = jnp.einsum('bhnts,bhnsv->bhntv', att, vc)
    lg = log_gamma[None, :, None, None, None]
    dS = jnp.einsum('bhnsk,bhnsv->bhnkv', kc * jnp.exp(lg * (C - 1.0 - pos)[:, None]), vc)
    chunk_decay = jnp.broadcast_to(jnp.exp(log_gamma * C)[None, :, None, None], (B, H, N, d))
    S_start = _chunk_state_scan(chunk_decay, dS)
    o = o + jnp.einsum('bhntk,bhnkv->bhntv', qc * jnp.exp(lg * (pos + 1.0)[:, None]), S_start)
    o = _from_chunks(o).reshape(B, T, H, d)
    mu = jnp.mean(o, axis=-1, keepdims=True)
    var = jnp.mean(jnp.square(o - mu), axis=-1, keepdims=True)
    o = (o - mu) * lax.rsqrt(var + EPS) * gn_w.astype(jnp.float32).reshape(H, d) + gn_b.astype(jnp.float32).reshape(H, d)
    o = o * jax.nn.silu(g.astype(jnp.float32)).reshape(B, T, H, d)
    return o.reshape(B, T, H * d).astype(dtype)


def _swa(q, k, v, qn_w, kn_w, sinks):
    B, T, _ = q.shape
    Q, KV, G, d = ATT_BLOCK, ATT_KV_HEADS, ATT_GROUP, HEAD_DIM
    N = T // Q
    dtype = q.dtype
    q = _rmsnorm(q.reshape(B, T, KV, G, d), qn_w).astype(jnp.float32)
    k = _rmsnorm(k.reshape(B, T, KV, d), kn_w).astype(jnp.float32)
    v = v.reshape(B, T, KV, d).astype(jnp.float32)
    qb = q.reshape(B, N, Q, KV, G, d)

    def with_prev(t):
        tb = t.reshape(B, N, Q, KV, d)
        prev = jnp.concatenate([jnp.zeros_like(tb[:, :1]), tb[:, :-1]], axis=1)
        return jnp.concatenate([prev, tb], axis=2)

    kw, vw = with_prev(k), with_prev(v)
    s = jnp.einsum('bnqkgd,bnskd->bnkgqs', qb, kw) * d ** -0.5
    dist = jnp.arange(Q)[:, None] + Q - jnp.arange(2 * Q)[None, :]
    valid = (dist >= 0) & (dist < WINDOW)
    valid = valid[None] & ((jnp.arange(N)[:, None, None] > 0) | (jnp.arange(2 * Q)[None, None, :] >= Q))
    slopes = jnp.exp2(-8.0 * jnp.arange(1, ATT_HEADS + 1, dtype=jnp.float32) / ATT_HEADS).reshape(KV, G, 1, 1)
    s = s - slopes * dist.astype(jnp.float32)
    s = jnp.where(valid[None, :, None, None], s, MASK_VALUE)
    sink = jnp.broadcast_to(sinks.astype(jnp.float32).reshape(1, 1, KV, G, 1, 1), s.shape[:-1] + (1,))
    p = jax.nn.softmax(jnp.concatenate([s, sink], axis=-1), axis=-1)[..., :-1]
    o = jnp.einsum('bnkgqs,bnskd->bnqkgd', p, vw)
    return o.reshape(B, T, KV * G * d).astype(dtype)


def _rglru(xb, gb, conv_w, conv_b, wa, ba, wx, bx, lam):
    B, T, W = xb.shape
    dtype = xb.dtype
    xf = xb.astype(jnp.float32)
    xp = jnp.pad(xf, ((0, 0), (CONV_WIDTH - 1, 0), (0, 0)))
    xc = conv_b.astype(jnp.float32)
    for j in range(CONV_WIDTH):
        xc = xc + xp[:, j:j + T] * conv_w[j].astype(jnp.float32)
    xr = xc.reshape(B, T, LRU_BLOCKS, LRU_BLOCK_DIM)
    r = jax.nn.sigmoid(jnp.einsum('btnc,nce->btne', xr, wa.astype(jnp.float32)).reshape(B, T, W) + ba.astype(jnp.float32))
    i = jax.nn.sigmoid(jnp.einsum('btnc,nce->btne', xr, wx.astype(jnp.float32)).reshape(B, T, W) + bx.astype(jnp.float32))
    log_a = -LRU_C * r * jax.nn.softplus(-lam.astype(jnp.float32))
    a = jnp.exp(log_a)
    u = jnp.sqrt(-jnp.expm1(2.0 * log_a)) * (i * xc)

    def combine(left, right):
        a_l, b_l = left
        a_r, b_r = right
        return a_l * a_r, a_r * b_l + b_r

    _, h = lax.associative_scan(combine, (a, u), axis=1)
    y = h * jax.nn.gelu(gb.astype(jnp.float32))
    return y.astype(dtype)


def setup_inputs(seed: int = 0) -> dict:
    key = jax.random.key(seed)
    kit = iter(jax.random.split(key, 40))
    L, D, F, W, bd = DEPTH, D_MODEL, D_FF, BRANCH_WIDTH, LRU_BLOCK_DIM

    def normal(shape, scale):
        return scale * jax.random.normal(next(kit), shape, jnp.float32)

    def gain(shape):
        return 1.0 + normal(shape, 0.02)

    x = normal((BATCH, SEQ, D), 1.0)
    ffn1_norm = gain((L, D))
    ffn1_wg = normal((L, D, F), D ** -0.5)
    ffn1_wu = normal((L, D, F), D ** -0.5)
    ffn1_wd = normal((L, F, D), F ** -0.5)
    mix_norm = gain((L, D))
    w_in = normal((L, D, D_IN), D ** -0.5)
    gate_bias = normal((L, N_BRANCH, D), 0.02)
    hgrn_lb_logits = normal((L, W), 0.5)
    hgrn_out_norm = gain((L, W))
    ret_gn_w = gain((L, W))
    ret_gn_b = normal((L, W), 0.02)
    attn_q_norm = gain((L, HEAD_DIM))
    attn_k_norm = gain((L, HEAD_DIM))
    attn_sinks = normal((L, ATT_HEADS), 1.0)
    lru_conv_w = normal((L, CONV_WIDTH, LRU_WIDTH), CONV_WIDTH ** -0.5)
    lru_conv_b = normal((L, LRU_WIDTH), 0.02)
    lru_wa = normal((L, LRU_BLOCKS, bd, bd), bd ** -0.5)
    lru_ba = normal((L, LRU_WIDTH), 0.02)
    lru_wx = normal((L, LRU_BLOCKS, bd, bd), bd ** -0.5)
    lru_bx = normal((L, LRU_WIDTH), 0.02)
    u = jax.random.uniform(next(kit), (L, LRU_WIDTH), jnp.float32, 0.9, 0.999)
    a0 = u ** (1.0 / LRU_C)
    lru_lambda = jnp.log(a0) - jnp.log1p(-a0)
    w_branch = normal((L, N_BRANCH, W, D), W ** -0.5)
    w_out = normal((L, D, D), D ** -0.5)
    ffn2_norm = gain((L, D))
    ffn2_wg = normal((L, D, F), D ** -0.5)
    ffn2_wu = normal((L, D, F), D ** -0.5)
    ffn2_wd = normal((L, F, D), F ** -0.5)
    return {
        "x": x, "ffn1_norm": ffn1_norm, "ffn1_wg": ffn1_wg, "ffn1_wu": ffn1_wu, "ffn1_wd": ffn1_wd,
        "mix_norm": mix_norm, "w_in": w_in, "gate_bias": gate_bias,
        "hgrn_lb_logits": hgrn_lb_logits, "hgrn_out_norm": hgrn_out_norm,
        "ret_gn_w": ret_gn_w, "ret_gn_b": ret_gn_b,
        "attn_q_norm": attn_q_norm, "attn_k_norm": attn_k_norm, "attn_sinks": attn_sinks,
        "lru_conv_w": lru_conv_w, "lru_conv_b": lru_conv_b, "lru_wa": lru_wa, "lru_ba": lru_ba,
        "lru_wx": lru_wx, "lru_bx": lru_bx, "lru_lambda": lru_lambda,
        "w_branch": w_branch, "w_out": w_out,
        "ffn2_norm": ffn2_norm, "ffn2_wg": ffn2_wg, "ffn2_wu": ffn2_wu, "ffn2_wd": ffn2_wd,
    }


def reference(x, ffn1_norm, ffn1_wg, ffn1_wu, ffn1_wd, mix_norm, w_in, gate_bias,
              hgrn_lb_logits, hgrn_out_norm, ret_gn_w, ret_gn_b,
              attn_q_norm, attn_k_norm, attn_sinks,
              lru_conv_w, lru_conv_b, lru_wa, lru_ba, lru_wx, lru_bx, lru_lambda,
              w_branch, w_out, ffn2_norm, ffn2_wg, ffn2_wu, ffn2_wd):
    B, T, D = x.shape
    lb_p = jax.nn.softmax(hgrn_lb_logits.astype(jnp.float32), axis=0)
    lower_bounds = jnp.cumsum(lb_p, axis=0) - lb_p[0:1]
    for l in range(DEPTH):
        x = x + 0.5 * _swiglu(_rmsnorm(x, ffn1_norm[l]), ffn1_wg[l], ffn1_wu[l], ffn1_wd[l])
        h = _rmsnorm(x, mix_norm[l])
        proj = h @ w_in[l]
        (hq, hf, hi, hg, rq, rk, rv, rg, aq, ak, av, lx, lg, gate_pre) = _split_cols(proj, IN_SIZES)
        y_a = _hgrn2(hq, hf, hi, hg, lower_bounds[l], hgrn_out_norm[l])
        y_b = _retention(rq, rk, rv, rg, ret_gn_w[l], ret_gn_b[l])
        y_c = _swa(aq, ak, av, attn_q_norm[l], attn_k_norm[l], attn_sinks[l])
        y_d = _rglru(lx, lg, lru_conv_w[l], lru_conv_b[l], lru_wa[l], lru_ba[l],
                     lru_wx[l], lru_bx[l], lru_lambda[l])
        gates = jax.nn.sigmoid(gate_pre.astype(jnp.float32).reshape(B, T, N_BRANCH, D)
                               + gate_bias[l].astype(jnp.float32)).astype(x.dtype)
        merged = gates[:, :, 0] * (y_a @ w_branch[l, 0])
        merged = merged + gates[:, :, 1] * (y_b @ w_branch[l, 1])
        merged = merged + gates[:, :, 2] * (y_c @ w_branch[l, 2])
        merged = merged + gates[:, :, 3] * (y_d @ w_branch[l, 3])
        x = x + merged @ w_out[l]
        x = x + 0.5 * _swiglu(_rmsnorm(x, ffn2_norm[l]), ffn2_wg[l], ffn2_wu[l], ffn2_wd[l])
    return x
```

```python
import numpy as np
import concourse.bass as bass
import concourse.mybir as mybir
from concourse.bass_utils import run_bass_kernel_spmd

F32 = mybir.dt.float32
BF16 = mybir.dt.bfloat16
AF = mybir.ActivationFunctionType
ALU = mybir.AluOpType

D = 1024
DFF = 2816
NL = 2
G = 512
NCH = 8
NHC = 22
EPS = 1e-6
ENGS = ("pe", "act", "dve", "pool", "sp")


class _Op:
    __slots__ = ("eng", "fn", "deps", "signal", "sigval", "dma_key", "dma_val")

    def __init__(self, eng, fn):
        self.eng = eng
        self.fn = fn
        self.deps = []
        self.signal = False
        self.sigval = 0
        self.dma_key = None
        self.dma_val = 0


class Sched:
    def __init__(self):
        self.ops = {e: [] for e in ENGS}
        self.last_w = {}
        self.readers = {}
        self.dma_cnt = {}
        self.batch_keys = set()

    def add(self, eng, fn, reads=(), writes=(), dma_key=None, batch=False):
        op = _Op(eng, fn)
        deps = []
        for r in reads:
            t = self.last_w.get(r)
            if t is not None:
                deps.append(t)
        for w in writes:
            t = self.last_w.get(w)
            if t is not None:
                deps.append(t)
            deps.extend(self.readers.get(w, ()))
        seen = set()
        for d in deps:
            if id(d) in seen or d is op:
                continue
            seen.add(id(d))
            if d.dma_key is None and d.eng == eng and eng == "pe":
                continue
            op.deps.append(d)
            if d.dma_key is None:
                d.signal = True
        if dma_key is not None:
            op.dma_key = dma_key
            self.dma_cnt[dma_key] = self.dma_cnt.get(dma_key, 0) + 16
            op.dma_val = self.dma_cnt[dma_key]
            if batch:
                self.batch_keys.add(dma_key)
        for r in reads:
            self.readers.setdefault(r, []).append(op)
        for w in writes:
            self.last_w[w] = op
            self.readers[w] = []
        self.ops[eng].append(op)
        return op

    def emit(self, nc, final_waits=()):
        for e in ENGS:
            n = 0
            for op in self.ops[e]:
                if op.dma_key is None and op.signal:
                    n += 1
                    op.sigval = n
        import contextlib
        with contextlib.ExitStack() as st:
            esem = {e: st.enter_context(nc.semaphore("s_" + e)) for e in ENGS}
            dsem = {k: st.enter_context(nc.semaphore("d_%s" % (str(k).replace(" ", "").replace("'", "").replace(",", "_").replace("(", "").replace(")", ""))))
                    for k in self.dma_cnt}
            block = st.enter_context(nc.Block())
            sched = self

            def run(e):
                def body(engine):
                    waited = {}
                    for op in sched.ops[e]:
                        for d in op.deps:
                            if d.dma_key is not None:
                                sem = dsem[d.dma_key]
                                val = sched.dma_cnt[d.dma_key] if d.dma_key in sched.batch_keys else d.dma_val
                                key = ("d", d.dma_key)
                            else:
                                sem = esem[d.eng]
                                val = d.sigval
                                key = ("e", d.eng)
                            if waited.get(key, 0) >= val:
                                continue
                            waited[key] = val
                            engine.wait_ge(sem, val)
                        ins = op.fn(engine)
                        if op.dma_key is not None:
                            ins.then_inc(dsem[op.dma_key], 16)
                        elif op.signal:
                            ins.then_inc(esem[e], 1)
                    if e == "sp":
                        for k in final_waits:
                            engine.wait_ge(dsem[k], sched.dma_cnt[k])
                return body

            block.tensor(run("pe"))
            block.scalar(run("act"))
            block.vector(run("dve"))
            block.gpsimd(run("pool"))
            block.sync(run("sp"))


CELL = 256


def _esz(dt):
    return 4 if dt == F32 else 2


def _cells(ap):
    name = ap.tensor.name
    if name in ("xT", "outT"):
        return [(name, ap.offset)]
    if name not in ("SB", "PS"):
        return [name]
    es = _esz(ap.dtype)
    pat = ap.ap
    pstep = pat[0][0]
    first = ap.offset % pstep if pstep else ap.offset
    last = first
    for st, cnt in pat[1:]:
        last += st * (cnt - 1)
    return [(name, c) for c in range(first * es // CELL, (last * es + es - 1) // CELL + 1)]


class Prog:
    def __init__(self, nc, sb_bytes):
        self.nc = nc
        self.S = Sched()
        self.SB = nc.alloc_sbuf_tensor("SB", [128, sb_bytes // 4], F32).ap()
        self.PS = nc.alloc_psum_tensor("PS", [128, 4096], F32).ap()
        self.sb_bytes = sb_bytes
        self.off = 0
        self.bank = 0

    def alloc(self, nbytes, dt, shape=None):
        nbytes = (nbytes + CELL - 1) // CELL * CELL
        o = self.off
        self.off += nbytes
        assert self.off <= self.sb_bytes, ("SBUF overflow", self.off)
        v = self.SB[:, o // 4:(o + nbytes) // 4]
        if dt != F32:
            v = v.bitcast(dt)
        return v

    def tile(self, dt, *shape):
        n = 1
        for s_ in shape:
            n *= s_
        v = self.alloc(n * _esz(dt), dt)[:, 0:n]
        if len(shape) == 2:
            return v.rearrange("p (a b) -> p a b", a=shape[0])
        if len(shape) == 3:
            return v.rearrange("p (a b c) -> p a b c", a=shape[0], b=shape[1])
        if len(shape) == 4:
            return v.rearrange("p (a b c d) -> p a b c d", a=shape[0], b=shape[1], c=shape[2])
        return v

    nrot = 4

    def psb(self, n=1):
        if self.bank + n > self.nrot:
            self.bank = 0
        b = self.bank
        self.bank = (self.bank + n) % self.nrot
        return self.PS[:, b * 512:(b + n) * 512]

    def psl(self, k):
        return self.PS[:, (4 + k) * 512:(5 + k) * 512]

    def op(self, eng, fn, outs, ins, dma_key=None, batch=False):
        r = []
        for a in ins:
            r.extend(_cells(a))
        w = []
        for a in outs:
            w.extend(_cells(a))
        return self.S.add(eng, fn, reads=r, writes=w, dma_key=dma_key, batch=batch)

    def mm(self, out, lhsT, rhs, start=True, stop=True):
        self.op("pe", lambda e: e.matmul(out, lhsT, rhs, start=start, stop=stop), [out], [lhsT, rhs] + ([] if start else [out]))

    def tr(self, out, in_, ident):
        self.op("pe", lambda e: e.transpose(out, in_, ident), [out], [in_, ident])

    def act(self, out, in_, func, bias=None, scale=None, eng="act"):
        ins = [in_]
        kw = {}
        if bias is not None:
            kw["bias"] = bias
            if not isinstance(bias, (int, float)):
                ins.append(bias)
        if scale is not None:
            kw["scale"] = scale
            if not isinstance(scale, (int, float)):
                ins.append(scale)
        self.op(eng, lambda e: e.activation(out, in_, func, **kw), [out], ins)

    def tt(self, out, a, b, op, eng="dve"):
        self.op(eng, lambda e: e.tensor_tensor(out, a, b, op), [out], [a, b])

    def ts(self, out, a, s1, s2, op0, op1=None, eng="dve"):
        ins = [a] + [s for s in (s1, s2) if s is not None and not isinstance(s, (int, float))]
        if op1 is None:
            self.op(eng, lambda e: e.tensor_scalar(out, a, s1, None, op0), [out], ins)
        else:
            self.op(eng, lambda e: e.tensor_scalar(out, a, s1, s2, op0, op1), [out], ins)

    def stt(self, out, a, sc, b, op0, op1, eng="dve"):
        ins = [a, b] + ([] if isinstance(sc, (int, float)) else [sc])
        self.op(eng, lambda e: e.scalar_tensor_tensor(out, a, sc, b, op0, op1), [out], ins)

    def copy(self, out, a, eng="dve"):
        self.op(eng, lambda e: e.tensor_copy(out, a), [out], [a])

    def memset(self, out, val, eng="pool"):
        self.op(eng, lambda e: e.memset(out, val), [out], [])

    def scan(self, out, d0, d1, init, op0, op1, eng="dve"):
        ins = [d0, d1] + ([] if isinstance(init, (int, float)) else [init])
        self.op(eng, lambda e: e.tensor_tensor_scan(out, d0, d1, init, op0, op1), [out], ins)

    def dma(self, eng, out, in_, key, batch=False):
        self.op(eng, lambda e: e.dma_start(out=out, in_=in_), [out], [in_], dma_key=key, batch=batch)


def _pc_layout():
    names = {}
    n = 0

    def add(key, cnt):
        nonlocal n
        names[key] = n
        n += cnt
    for l in range(NL):
        add(("ffn1_norm", l), 8)
        add(("mix_norm", l), 8)
        add(("ffn2_norm", l), 8)
        add(("gate_bias", l), 32)
        add(("lb_logit", l), 2)
        add(("hg_outw", l), 2)
        add(("gn_w", l), 2)
        add(("gn_b", l), 2)
        add(("qn_w", l), 1)
        add(("kn_w", l), 1)
        add(("sink", l), 2)
        add(("conv_w", l), 8)
        add(("conv_b", l), 2)
        add(("lru_ba", l), 2)
        add(("lru_bx", l), 2)
        add(("lru_lam", l), 2)
    return names, n


PC, NPC = _pc_layout()


def _fm(v):
    return np.ascontiguousarray(np.asarray(v, np.float32).reshape(-1, 128).T)


class Builder:
    def __init__(self, T, phases=("ffn1", "mix", "ffn2"), n_layers=NL, dbg=None):
        self.T = T
        self.NG = T // G
        self.phases = phases
        self.n_layers = n_layers
        self.dbg = dbg
        nc = self.nc = bass.Bass("TRN2", target_bir_lowering=False)
        self.P = P = Prog(nc, 207 * 1024)
        dt = nc.dram_tensor
        self.x_in = dt("xT", [D, T], F32, kind="ExternalInput").ap()
        self.out = dt("outT", [D, T], F32, kind="ExternalOutput").ap()
        self.pcols_d = dt("pcols", [128, NPC], F32, kind="ExternalInput").ap()
        self.w32 = {}
        self.wbf = {}
        for nm, shp in (("wg1", [D, DFF]), ("wu1", [D, DFF]), ("wd1", [DFF, D]),
                        ("win", [D, WIN_COLS]), ("wbr", [1024, D]), ("wout", [D, D]),
                        ("wg2", [D, DFF]), ("wu2", [D, DFF]), ("wd2", [DFF, D])):
            for l in range(n_layers):
                self.w32[(nm, l)] = dt("%s_%d" % (nm, l), shp, F32, kind="ExternalInput").ap()
                self.wbf[(nm, l)] = dt("b_%s_%d" % (nm, l), shp, BF16, kind="Internal").ap()
        self.xT = P.tile(F32, NCH, G)
        self.hT = P.tile(BF16, NCH, G)
        self.pcols = P.tile(F32, 1, NPC)[:, 0, :]
        self.big = P.alloc(32768, BF16)
        self.sqb = self.big[:, 0:NCH * G].rearrange("p (c n) -> p c n", c=NCH)
        self.wslots = [P.alloc(8192, BF16) for _ in range(3)]
        self.wslot_i = 0
        self.m_a, self.m_b, self.m_c, self.m_d, self.m_e = [P.tile(F32, 1, G)[:, 0, :] for _ in range(5)]
        self.rstd = self.m_e
        self.onesD = P.tile(BF16, 1, 128)[:, 0, :]
        self.cst32 = P.tile(F32, 1, 128)[:, 0, :]

    def pc(self, key, j=0, n=1):
        o = PC[key] + j
        return self.pcols[:, o:o + n]

    def wslot(self):
        s = self.wslots[self.wslot_i]
        self.wslot_i = (self.wslot_i + 1) % len(self.wslots)
        return s

    def wload(self, dst, src):
        self.P.dma("sp", dst, src, key=("w", id(dst.tensor), dst.offset))

    def convert_all(self):
        P = self.P
        order = ("wg1", "wu1", "wd1", "win", "wbr", "wout", "wg2", "wu2", "wd2")
        for l in range(self.n_layers):
            for nm in order:
                P.dma("pool", self.wbf[(nm, l)], self.w32[(nm, l)], key=("cv", nm, l))

    def consts(self):
        P = self.P
        P.dma("act", self.pcols, self.pcols_d, key="c0")
        P.memset(self.cst32, 1.0 / D)
        P.copy(self.onesD, self.cst32, eng="pool")

    def rmsnorm(self, wkey, l):
        P = self.P
        ps = P.psb()
        for c in range(NCH):
            P.act(self.sqb[:, c, :], self.xT[:, c, :], AF.Square)
        for c in range(NCH):
            P.mm(ps, self.onesD, self.sqb[:, c, :], start=(c == 0), stop=(c == NCH - 1))
        P.act(self.rstd, ps, AF.Ln, bias=self.eps_col)
        P.act(self.rstd, self.rstd, AF.Exp, scale=-0.5)
        for c in range(NCH):
            P.stt(self.hT[:, c, :], self.xT[:, c, :], self.pc((wkey, l), c), self.rstd, ALU.mult, ALU.mult)

    def ffn(self, l, which):
        P = self.P
        wg, wu, wd = self.wbf[("wg" + which, l)], self.wbf[("wu" + which, l)], self.wbf[("wd" + which, l)]
        self.rmsnorm("ffn%s_norm" % which, l)
        actT = self.big[:, 0:NHC * G].rearrange("p (j n) -> p j n", j=NHC)
        wgv = wg.rearrange("(c p) n -> p c n", p=128)
        wuv = wu.rearrange("(c p) n -> p c n", p=128)
        P.nrot = 8
        P.bank = 0
        for jp in range(NHC // 2):
            slot = self.wslot()
            wt = slot.rearrange("p (u c n) -> p u c n", u=2, c=NCH)
            self.wload(wt[:, 0], wgv[:, :, jp * 256:(jp + 1) * 256])
            self.wload(wt[:, 1], wuv[:, :, jp * 256:(jp + 1) * 256])
            for jj in range(2):
                j = jp * 2 + jj
                pg = P.psb()
                pu = P.psb()
                for c in range(NCH):
                    P.mm(pg, wt[:, 0, c, jj * 128:(jj + 1) * 128], self.hT[:, c, :], start=(c == 0), stop=(c == NCH - 1))
                for c in range(NCH):
                    P.mm(pu, wt[:, 1, c, jj * 128:(jj + 1) * 128], self.hT[:, c, :], start=(c == 0), stop=(c == NCH - 1))
                sg = self.tmps[j % 3]
                P.act(sg, pg, AF.Silu)
                P.tt(actT[:, j, :], pu, sg, ALU.mult)
        P.nrot = 4
        P.bank = 0
        wdv = wd.rearrange("(j p) n -> p j n", p=128)
        for half in range(2):
            pss = [P.psl(k_) for k_ in range(4)]
            j0 = 0
            for nj in (8, 8, 6):
                slot = self.wslot()
                wt = slot.rearrange("p (j n) -> p j n", j=8)
                self.wload(wt[:, 0:nj], wdv[:, j0:j0 + nj, half * 512:(half + 1) * 512])
                for jj in range(nj):
                    j = j0 + jj
                    for m in range(4):
                        P.mm(pss[m], wt[:, jj, m * 128:(m + 1) * 128], actT[:, j, :], start=(j == 0), stop=(j == NHC - 1))
                j0 += nj
            for m in range(4):
                c = half * 4 + m
                P.stt(self.xT[:, c, :], pss[m], 0.5, self.xT[:, c, :], ALU.mult, ALU.add)

    def build(self):
        P = self.P
        self.eps_col = P.tile(F32, 1, 1)[:, 0, :]
        self.tmps = [self.m_a, self.m_b, self.m_c]
        P.memset(self.eps_col, EPS)
        self.consts()
        self.convert_all()
        if "mix" in self.phases:
            self.mixer_setup()
        xv = self.x_in.rearrange("(c p) t -> p c t", p=128)
        ov = self.out.rearrange("(c p) t -> p c t", p=128)
        for g in range(self.NG):
            for cx in range(NCH):
                P.dma("pool", self.xT[:, cx, :], xv[:, cx, g * G:(g + 1) * G], key=("xin", cx))
            for l in range(self.n_layers):
                if "ffn1" in self.phases:
                    self.ffn(l, "1")
                if "mix" in self.phases:
                    self.mixer(l, g)
                if "ffn2" in self.phases:
                    self.ffn(l, "2")
            for cx in range(NCH):
                P.dma("pool", ov[:, cx, g * G:(g + 1) * G], self.xT[:, cx, :], key=("xout", cx))
        P.S.emit(self.nc, final_waits=[("xout", cx) for cx in range(NCH)])
        return self.nc


WIN_COLS = 51 * 128 + 896


def _win_perm():
    o = {}
    names = ("hq", "hf", "hi", "hg", "rq", "rk", "rv", "rg", "aq", "ak", "av", "lx", "lg", "gate")
    sizes = (256, 256, 256, 256, 256, 256, 256, 256, 256, 128, 128, 256, 256, 4096)
    s = 0
    for n_, z in zip(names, sizes):
        o[n_] = s
        s += z
    r = lambda n_, a=0, b=None: list(range(o[n_] + a, o[n_] + (b if b is not None else dict(zip(names, sizes))[n_])))
    cols = []
    cols += r("gate")
    cols += r("hq") + r("hf") + r("hg")
    cols += r("rq") + r("rk") + r("rg")
    cols += r("aq", 0, 64) + r("aq", 128, 192) + r("aq", 64, 128) + r("aq", 192, 256)
    cols += r("ak")
    cols += r("lx") + r("lg")
    cols += r("rk") + r("rv") + r("hi") + r("av")
    return np.array(cols, np.int64)


def host_inputs(inp, b, T=None):
    x = np.asarray(inp["x"])[b]
    if T is not None:
        x = x[:T]
    m = {"xT": np.ascontiguousarray(x.T)}
    pc = np.zeros((128, NPC), np.float32)
    perm = _win_perm()
    for l in range(NL):
        def put(key, arr):
            a = _fm(arr)
            pc[:, PC[(key, l)]:PC[(key, l)] + a.shape[1]] = a
        put("ffn1_norm", inp["ffn1_norm"][l])
        put("mix_norm", inp["mix_norm"][l])
        put("ffn2_norm", inp["ffn2_norm"][l])
        put("gate_bias", np.asarray(inp["gate_bias"][l]).reshape(-1))
        put("lb_logit", inp["hgrn_lb_logits"][l])
        put("hg_outw", inp["hgrn_out_norm"][l])
        put("gn_w", inp["ret_gn_w"][l])
        put("gn_b", inp["ret_gn_b"][l])
        put("qn_w", np.tile(np.asarray(inp["attn_q_norm"][l]), 2))
        put("kn_w", np.tile(np.asarray(inp["attn_k_norm"][l]), 2))
        put("sink", np.repeat(np.asarray(inp["attn_sinks"][l]), 64))
        cw = np.asarray(inp["lru_conv_w"][l])
        put("conv_w", np.concatenate([cw[:, 0:128].reshape(-1), cw[:, 128:256].reshape(-1)]).reshape(8, 128).reshape(-1))
        put("conv_b", inp["lru_conv_b"][l])
        put("lru_ba", inp["lru_ba"][l])
        put("lru_bx", inp["lru_bx"][l])
        put("lru_lam", inp["lru_lambda"][l])
        m["wg1_%d" % l] = np.asarray(inp["ffn1_wg"][l])
        m["wu1_%d" % l] = np.asarray(inp["ffn1_wu"][l])
        m["wd1_%d" % l] = np.asarray(inp["ffn1_wd"][l])
        m["wg2_%d" % l] = np.asarray(inp["ffn2_wg"][l])
        m["wu2_%d" % l] = np.asarray(inp["ffn2_wu"][l])
        m["wd2_%d" % l] = np.asarray(inp["ffn2_wd"][l])
        m["win_%d" % l] = np.ascontiguousarray(np.asarray(inp["w_in"][l])[:, perm])
        m["wbr_%d" % l] = np.ascontiguousarray(np.asarray(inp["w_branch"][l]).reshape(1024, D))
        m["wout_%d" % l] = np.asarray(inp["w_out"][l])
    m["pcols"] = pc
    return m


def _ct_layout():
    names = {}
    n = 0

    def add(key, cnt):
        nonlocal n
        names[key] = (n, cnt)
        n += cnt
    add("ident", 128)
    add("blk64", 128)
    add("bdmask", 128)
    add("reset", G)
    add("cmask8", 8)
    add("hmaskT2", 256)
    for p in range(2):
        add(("decayT", p), 256)
        add(("qdecay", p), 128)
    add("kdecay", 256)
    add("g128", 2)
    return names, n


CT, NCT = _ct_layout()
NCB = 128 + 128 + 9 * 128 + 256


def const_table():
    t = np.zeros((128, NCT), np.float64)

    def put(key, arr):
        o, c = CT[key]
        t[:, o:o + c] = arr
    idx = np.arange(128)
    put("ident", np.eye(128))
    blk = (idx[:, None] // 64 == idx[None, :] // 64).astype(np.float64)
    put("blk64", blk / 64.0)
    put("bdmask", blk)
    tt = np.arange(G)
    put("reset", np.broadcast_to((tt % 16 != 0).astype(np.float64), (128, G)))
    put("cmask8", (idx[:, None] // 16 == np.arange(8)[None, :]).astype(np.float64))
    hm = ((idx[:, None] // 16 == idx[None, :] // 16) & (idx[:, None] <= idx[None, :])).astype(np.float64)
    put("hmaskT2", np.concatenate([hm, hm], 1))
    lg = np.log1p(-np.exp2(-5.0 - np.arange(4)))
    rel = idx[None, :] - idx[:, None]
    kd = np.zeros((128, 256))
    g128 = np.zeros((128, 2))
    for p in range(2):
        dT = []
        qd = np.zeros((128, 128))
        for hh in range(2):
            h = 2 * p + hh
            dT.append(np.where(rel >= 0, np.exp(lg[h] * np.maximum(rel, 0)), 0.0) / 8.0)
            qd[hh * 64:(hh + 1) * 64, :] = np.exp(lg[h] * (idx[None, :] + 1.0)) / 8.0
            kd[:, h * 64:(h + 1) * 64] = np.exp(lg[h] * (127.0 - idx))[:, None]
            g128[hh * 64:(hh + 1) * 64, p] = np.exp(lg[h] * 128.0)
        put(("decayT", p), np.concatenate(dT, 1))
        put(("qdecay", p), qd)
    put("kdecay", kd)
    put("g128", g128)
    tb = np.zeros((128, NCB), np.float64)
    tb[:, 0:128] = np.eye(128)
    tb[:, 128:256] = blk / 64.0
    slopes = np.exp2(-8.0 * np.arange(1, 5) / 4.0)
    NEG = -240000.0
    kk = idx[:, None]
    qq = idx[None, :]
    for h in range(4):
        prev = np.where(kk > qq, -slopes[h] * (qq + 128 - kk) * 8.0, NEG)
        cur = np.where(kk <= qq, -slopes[h] * (qq - kk) * 8.0, NEG)
        tb[:, 256 + (2 * h) * 128:256 + (2 * h + 1) * 128] = prev
        tb[:, 256 + (2 * h + 1) * 128:256 + (2 * h + 2) * 128] = cur
    tb[:, 256 + 8 * 128:256 + 9 * 128] = NEG
    op = np.zeros((128, 256))
    op[:, 0:64] = 1.0
    op[:, 128 + 64:256] = 1.0
    tb[:, 256 + 9 * 128:] = op
    return t.astype(np.float32), tb.astype(np.float32)


NB = G // 128
import os
SKIP = os.environ.get('SKIP', '')
STOP = int(os.environ.get('STOP', '99'))
FMC = 19


def _mixer_setup(self):
    P = self.P
    nc = self.nc
    self.ctab_d = nc.dram_tensor("ctab", [128, NCT], F32, kind="ExternalInput").ap()
    self.lruw_d = nc.dram_tensor("lruw", [NL, 4, 128, 128], F32, kind="ExternalInput").ap()
    self.ct = P.tile(F32, 1, NCT)[:, 0, :]
    P.dma("act", self.ct, self.ctab_d, key="c3")
    c = lambda key: self.ct[:, CT[key][0]:CT[key][0] + CT[key][1]]
    self.c = c
    self.cbf_d = nc.dram_tensor("cbf", [128, NCB], F32, kind="ExternalInput").ap()
    cb = P.tile(BF16, 1, NCB)[:, 0, :]
    P.dma("pool", cb, self.cbf_d, key="c1")
    self.identb = cb[:, 0:128]
    self.blk64b = cb[:, 128:256]
    self.biasb = cb[:, 256:256 + 9 * 128].rearrange("p (a n) -> p a n", a=9)
    self.onespad = cb[:, 256 + 9 * 128:].rearrange("p (a n) -> p a n", a=2)
    self.lruw = P.tile(BF16, NL * 4, 128)
    P.dma("pool", self.lruw, self.lruw_d.rearrange("l j p n -> p (l j) n"), key="c2")
    self.dcols = P.tile(F32, NL, 16)
    for l in range(self.n_layers):
        dc = self.dcols[:, l, :]
        if l == 0:
            P.memset(dc[:, 0:2], 0.0)
        else:
            P.tt(dc[:, 10:12], self.pc(("lb_logit", 1), 0, 2), self.pc(("lb_logit", 0), 0, 2), ALU.subtract)
            P.act(dc[:, 0:2], dc[:, 10:12], AF.Sigmoid)
        P.ts(dc[:, 2:4], dc[:, 0:2], -1.0, 1.0, ALU.mult, ALU.add)
        P.act(dc[:, 4:6], self.pc(("sink", l), 0, 2), AF.Exp)
        P.act(dc[:, 12:14], self.pc(("lru_lam", l), 0, 2), AF.Exp, scale=-1.0)
        P.act(dc[:, 12:14], dc[:, 12:14], AF.Ln, bias=1.0)
        P.ts(dc[:, 6:8], dc[:, 12:14], -8.0, None, ALU.mult)
        P.ts(dc[:, 8:10], dc[:, 12:14], -16.0, None, ALU.mult)
    L = self.n_layers
    self.Sh = P.tile(F32, 9, 128)
    self.Sc = P.tile(F32, L * 2, 128)
    self.Sr = P.tile(F32, L * 2, 128)
    self.khat = P.tile(BF16, L, 128 + G)
    self.vpad = P.tile(BF16, NB + 1, 4 * 128)
    self.vcar = P.tile(BF16, L, 4 * 128)
    self.lxh = P.tile(F32, 2, G + 4)
    self.lxc = P.tile(F32, L * 2, 4)
    self.hst = P.tile(F32, L, 2)
    for t_ in (self.Sh, self.Sc, self.Sr, self.khat, self.vpad, self.vcar, self.lxh, self.lxc, self.hst):
        P.memset(t_, 0.0)
    self.gatesb = self.big[:, 0:32 * G].rearrange("p (j n) -> p j n", j=32)
    f32t = lambda *s: P.tile(F32, *s)
    bft = lambda *s: P.tile(BF16, *s)
    self.m_q = f32t(2, G)
    self.m_z = f32t(2, G)
    self.m_sg = f32t(2, G)
    self.m_qd = f32t(2, G)
    self.m_kpp = self.m_e
    self.m_qt = bft(2, G)
    self.m_kt = bft(2, G)
    self.m_dcol = f32t(2, 32)
    self.m_km8 = bft(8, 128)
    self.m_dSm = f32t(8, 128)
    self.m_att = bft(2, 256)
    self.m_tok = bft(NB, 256)
    self.m_vtok = bft(NB, 256)
    self.m_pad = bft(NB, 4 * 128)
    self.m_tmp128 = f32t(1, 128)[:, 0, :]
    self.m_lg = self.m_q
    self.hi_tok = bft(NB, 256)
    self.hi_pad = bft(NB, 4 * 128)
    self.gslots = [P.alloc(4096, BF16) for _ in range(2)]
    self.r_a, self.r_b, self.r_c, self.r_d = [f32t(1, G)[:, 0, :] for _ in range(4)]
    self.r_qd = f32t(2, G)
    self.r_qt = bft(2, G)
    self.r_kt = bft(2, G)
    self.r_sg = bft(2, G)
    self.r_att = bft(2, 256)
    P.memset(self.m_pad, 0.0)
    P.memset(self.hi_pad, 0.0)
    self.yT = [bft(2, G) for _ in range(4)]
    self.merged = self.hT
    self.acc = [self.m_a, self.m_b, self.m_c, self.m_d]


def _gates_some(self, l, k):
    P = self.P
    winv = self.wbf[("win", l)].rearrange("(c p) n -> p c n", p=128)
    while k > 0 and self.g_next < 32:
        j = self.g_next
        if j % 2 == 0:
            wt_ = self.gslots[(j // 2) % 2].rearrange("p (c n) -> p c n", c=NCH)
            self.wload(wt_, winv[:, :, j * 128:(j + 2) * 128])
            self.gate_slot = wt_
        wt = self.gate_slot
        ps = P.psb()
        cc = j % 2
        for c in range(NCH):
            P.mm(ps, wt[:, c, cc * 128:(cc + 1) * 128], self.hT[:, c, :], start=(c == 0), stop=(c == NCH - 1))
        P.act(self.gatesb[:, j, :], ps, AF.Sigmoid, bias=self.pc(("gate_bias", l), j))
        self.g_next += 1
        k -= 1


def _fm_load(self, l, m0, n):
    winv = self.wbf[("win", l)].rearrange("(c p) n -> p c n", p=128)
    wt = self.wslot().rearrange("p (c n) -> p c n", c=NCH)
    c0 = (32 + m0) * 128
    self.wload(wt[:, :, 0:n * 128], winv[:, :, c0:c0 + n * 128])
    return wt


def _proj(self, wt, k):
    P = self.P
    ps = P.psb()
    for cc in range(NCH):
        P.mm(ps, wt[:, cc, k * 128:(k + 1) * 128], self.hT[:, cc, :], start=(cc == 0), stop=(cc == NCH - 1))
    return ps


def _mixer(self, l, g):
    P = self.P
    c = self.c
    self.rmsnorm("mix_norm", l)
    winv = self.wbf[("win", l)].rearrange("(c p) n -> p c n", p=128)
    TM0 = 51 * 128
    slot0 = self.wslot().rearrange("p (c n) -> p c n", c=NCH)
    self.wload(slot0, winv[:, :, TM0:TM0 + 512])
    slot1 = self.wslot().rearrange("p (c n) -> p c n", c=NCH)
    self.wload(slot1[:, :, 0:384], winv[:, :, TM0 + 512:TM0 + 896])
    self.wload(slot1[:, :, 384:512], winv[:, :, TM0 + 768:TM0 + 896])
    vp = self.vpad
    P.copy(vp[:, 0, :], self.vcar[:, l, :], eng="pool")
    mpad = self.m_pad
    for i in range(NB):
        p0 = P.psb()
        p1 = P.psb()
        for cc in range(NCH):
            P.mm(p0, self.hT[:, cc, i * 128:(i + 1) * 128], slot0[:, cc, :], start=(cc == 0), stop=(cc == NCH - 1))
        for cc in range(NCH):
            P.mm(p1, self.hT[:, cc, i * 128:(i + 1) * 128], slot1[:, cc, :], start=(cc == 0), stop=(cc == NCH - 1))
        P.tt(self.m_tok[:, i, :], p0[:, 0:256], c("kdecay"), ALU.mult)
        P.copy(self.m_vtok[:, i, :], p0[:, 256:512])
        for v_ in range(4):
            side = v_ % 2
            dst_, src_ = mpad[:, i, v_ * 128 + side * 64:v_ * 128 + side * 64 + 64], p0[:, 256 + v_ * 64:256 + v_ * 64 + 64]
            P.copy(dst_, src_)
        P.copy(self.hi_tok[:, i, :], p1[:, 0:256])
        for v_ in range(4):
            side = v_ % 2
            dst_, src_ = self.hi_pad[:, i, v_ * 128 + side * 64:v_ * 128 + side * 64 + 64], p1[:, v_ * 64:v_ * 64 + 64]
            P.copy(dst_, src_)
        for v_ in range(4):
            side = v_ % 2
            kv_ = v_ // 2
            dst_, src_ = vp[:, i + 1, v_ * 128 + side * 64:v_ * 128 + side * 64 + 64], p1[:, 256 + kv_ * 64:256 + kv_ * 64 + 64]
            P.copy(dst_, src_)
    self.g_next = 0

    def chain(*gens):
        for g_ in gens:
            yield from g_
    lru0 = self.rglru(l, g, 0, (self.m_a, self.m_b, self.m_c, self.m_d, self.m_e), self.m_q[:, 0, :], self.m_kt[:, 0, :])
    lru1 = self.rglru(l, g, 1, (self.r_a, self.r_b, self.r_c, self.r_d, self.r_qd[:, 0, :]), self.r_qd[:, 1, :], self.r_kt[:, 1, :])
    streams = [chain(self.hgrn(l, g), lru0), chain(self.retention(l, g), self.swa(l, g), lru1)]
    while streams:
        for s_ in list(streams):
            try:
                next(s_)
            except StopIteration:
                streams.remove(s_)
    self._gates_some(l, 32)
    self.merge(l, g)


def _rs(self, out, ps):
    P = self.P
    P.act(out, ps, AF.Ln, bias=self.eps_col)
    P.act(out, out, AF.Exp, scale=-0.5)


def _hgrn(self, l, g):
    P = self.P
    c = self.c
    dc = self.dcols[:, l, :]
    a_, b_, c_, d_, e_ = self.m_a, self.m_b, self.m_c, self.m_d, self.m_e
    yT = self.yT[0]
    wt = self._fm_load(l, 0, 4)
    for p in range(2):
        P.copy(self.m_q[:, p, :], self._proj(wt, p))
    for p in range(2):
        P.act(self.m_z[:, p, :], self._proj(wt, 2 + p), AF.Sigmoid)
    wt = self._fm_load(l, 4, 2)
    for p in range(2):
        P.act(self.m_sg[:, p, :], self._proj(wt, p), AF.Sigmoid)
    yield
    for p in range(2):
        q = self.m_q[:, p, :]
        f = self.m_z[:, p, :]
        Sh = self.Sh
        P.copy(Sh[:, 0, :], self.Sc[:, l * 2 + p, :], eng="pool")
        P.ts(f, f, dc[:, 2 + p:3 + p], dc[:, p:p + 1], ALU.mult, ALU.add)
        P.ts(a_, f, -1.0, 1.0, ALU.mult, ALU.add)
        P.act(e_, f, AF.Ln)
        P.scan(b_, c("reset"), e_, 0.0, ALU.mult, ALU.add)
        yield
        b3 = b_.rearrange("p (n j) -> p n j", j=16)
        P.tt(c_.rearrange("p (n j) -> p n j", j=16), b3, b3[:, :, 8:9].to_broadcast([128, G // 16, 16]), ALU.subtract)
        P.act(d_, c_, AF.Exp)
        P.tt(self.m_qt[:, p, :], q, d_, ALU.mult)
        yield
        P.act(d_, c_, AF.Exp, scale=-1.0)
        P.tt(self.m_kt[:, p, :], a_, d_, ALU.mult)
        P.act(d_, b_, AF.Exp)
        P.tt(self.m_qd[:, p, :], q, d_, ALU.mult)
        yield
        P.tt(c_.rearrange("p (n j) -> p n j", j=16), b3[:, :, 15:16].to_broadcast([128, G // 16, 16]), b3, ALU.subtract)
        P.act(d_, c_, AF.Exp)
        P.tt(self.m_kpp, a_, d_, ALU.mult)
        P.act(self.m_dcol[:, p, :], b3[:, :, 15], AF.Exp)
        yield
        o_all = P.psl(p)
        for i in range(NB):
            bs = slice(i * 128, (i + 1) * 128)
            kT = P.psb()
            P.tr(kT[:, 0:128], self.m_kpp[:, bs], c("ident"))
            P.tt(self.m_km8, kT[:, 0:128].unsqueeze(1).to_broadcast([128, 8, 128]),
                 c("cmask8").unsqueeze(2).to_broadcast([128, 8, 128]), ALU.mult)
            yield
            dS = P.psb(2)
            for cc in range(8):
                P.mm(dS[:, cc * 128:(cc + 1) * 128], self.m_km8[:, cc, :], self.hi_tok[:, i, p * 128:(p + 1) * 128])
            for hb in range(2):
                P.tt(self.m_dSm[:, hb * 4:(hb + 1) * 4, :], dS[:, hb * 512:(hb + 1) * 512].rearrange("p (a n) -> p a n", a=4),
                     c("bdmask").unsqueeze(1).to_broadcast([128, 4, 128]), ALU.mult)
            yield
            for cc in range(8):
                P.stt(Sh[:, cc + 1, :], Sh[:, cc, :], self.m_dcol[:, p, i * 8 + cc:i * 8 + cc + 1], self.m_dSm[:, cc, :], ALU.mult, ALU.add)
            aT = P.psb(2)
            for hh in range(2):
                rs_ = slice(hh * 64, (hh + 1) * 64)
                P.mm(aT[:, hh * 512:hh * 512 + 128], self.m_kt[rs_, p, bs], self.m_qt[rs_, p, bs])
            att = self.m_att[:, i % 2, :]
            P.tt(att.rearrange("p (a n) -> p a n", a=2), aT.rearrange("p (a n) -> p a n", a=2)[:, :, 0:128],
                 c("hmaskT2").rearrange("p (a n) -> p a n", a=2), ALU.mult)
            yield
            hp = self.hi_pad[:, i, :].rearrange("p (a s n) -> p a s n", a=2, s=2)
            P.mm(o_all[:, bs], hp[:, p, 0, :], att[:, 0:128], start=True, stop=False)
            P.mm(o_all[:, bs], hp[:, p, 1, :], att[:, 128:256], start=False, stop=False)
            for cc in range(8):
                cs = slice(i * 128 + cc * 16, i * 128 + cc * 16 + 16)
                P.mm(o_all[:, cs], Sh[:, cc, :], self.m_qd[:, p, cs], start=False, stop=(cc == 7))
            P.copy(Sh[:, 0, :], Sh[:, 8, :], eng="pool")
            self._gates_some(l, 1)
            yield
        P.copy(self.Sc[:, l * 2 + p, :], Sh[:, 0, :], eng="pool")
        sq = self.m_kt[:, p, :]
        P.act(sq, o_all, AF.Square)
        ms = P.psb()
        P.mm(ms, self.blk64b, sq)
        self._rs(d_, ms)
        yield
        P.stt(c_, o_all, self.pc(("hg_outw", l), p), d_, ALU.mult, ALU.mult)
        P.tt(yT[:, p, :], c_, self.m_sg[:, p, :], ALU.mult)
        yield


def _retention(self, l, g):
    P = self.P
    c = self.c
    yT = self.yT[1]
    a_, b_, c_, d_ = self.r_a, self.r_b, self.r_c, self.r_d
    qt, kt, qd, sg = self.r_qt, self.r_kt, self.r_qd, self.r_sg
    wt = self._fm_load(l, 6, 4)
    for p in range(2):
        ps = self._proj(wt, p)
        P.copy(qt[:, p, :], ps)
        P.tt(qd[:, p, :].rearrange("p (b t) -> p b t", b=NB), ps.rearrange("p (b t) -> p b t", b=NB),
             c(("qdecay", p)).unsqueeze(1).to_broadcast([128, NB, 128]), ALU.mult)
    for p in range(2):
        P.copy(kt[:, p, :], self._proj(wt, 2 + p))
    wt = self._fm_load(l, 10, 2)
    for p in range(2):
        P.act(sg[:, p, :], self._proj(wt, p), AF.Silu)
    yield
    o_alls = [P.psl(2), P.psl(3)]
    for i in range(NB):
        bs = slice(i * 128, (i + 1) * 128)
        for p in range(2):
            S_ = self.Sr[:, l * 2 + p, :]
            aT = P.psb(2)
            for hh in range(2):
                rs_ = slice(hh * 64, (hh + 1) * 64)
                P.mm(aT[:, hh * 512:hh * 512 + 128], kt[rs_, p, bs], qt[rs_, p, bs])
            att = self.r_att[:, (i * 2 + p) % 2, :]
            P.tt(att.rearrange("p (a n) -> p a n", a=2), aT.rearrange("p (a n) -> p a n", a=2)[:, :, 0:128],
                 c(("decayT", p)).rearrange("p (a n) -> p a n", a=2), ALU.mult)
            yield
            vp = self.m_pad[:, i, :].rearrange("p (a s n) -> p a s n", a=2, s=2)
            P.mm(o_alls[p][:, bs], vp[:, p, 0, :], att[:, 0:128], start=True, stop=False)
            P.mm(o_alls[p][:, bs], vp[:, p, 1, :], att[:, 128:256], start=False, stop=False)
            P.mm(o_alls[p][:, bs], S_, qd[:, p, bs], start=False, stop=True)
            dS = P.psb()
            P.mm(dS[:, 0:128], self.m_tok[:, i, p * 128:(p + 1) * 128], self.m_vtok[:, i, p * 128:(p + 1) * 128])
            P.tt(self.m_tmp128, dS[:, 0:128], c("bdmask"), ALU.mult)
            P.stt(S_, S_, c("g128")[:, p:p + 1], self.m_tmp128, ALU.mult, ALU.add)
            yield
        self._gates_some(l, 1)
    for p in range(2):
        o_ps = o_alls[p]
        P.act(a_, o_ps, AF.Square) if False else P.copy(a_, o_ps)
        obf = kt[:, p, :]
        P.copy(obf, o_ps)
        mean = P.psb()
        P.mm(mean, self.blk64b, obf)
        P.tt(b_, a_, mean, ALU.subtract)
        yield
        sq = qt[:, p, :]
        P.act(sq, b_, AF.Square)
        var = P.psb()
        P.mm(var, self.blk64b, sq)
        self._rs(d_, var)
        yield
        P.tt(c_, b_, d_, ALU.mult)
        P.ts(c_, c_, self.pc(("gn_w", l), p), self.pc(("gn_b", l), p), ALU.mult, ALU.add)
        P.tt(yT[:, p, :], c_, sg[:, p, :], ALU.mult)
        yield


def _swa(self, l, g):
    P = self.P
    c = self.c
    dc = self.dcols[:, l, :]
    yT = self.yT[2]
    a_, b_, c_, d_ = self.r_a, self.r_b, self.r_c, self.r_d
    qh = self.r_qt
    khat = self.khat[:, l, :]
    vp = self.vpad
    wt = self._fm_load(l, 12, 3)
    raws = [a_, b_, c_]
    for k_ in range(3):
        P.copy(raws[k_], self._proj(wt, k_))
    yield
    for which in range(3):
        raw = raws[which]
        wcol = self.pc(("qn_w", l)) if which < 2 else self.pc(("kn_w", l))
        dst = qh[:, which, :] if which < 2 else khat[:, 128:128 + G]
        sq = self.r_kt[:, which % 2, :]
        P.act(sq, raw, AF.Square)
        ms = P.psb()
        P.mm(ms, self.blk64b, sq)
        self._rs(d_, ms)
        P.stt(dst, raw, wcol, d_, ALU.mult, ALU.mult)
        yield
    n_att = 0
    for j in range(2):
        o_all = P.psl(2)
        den = P.psl(3)
        for i in range(NB):
            bs = slice(i * 128, (i + 1) * 128)
            first = (g == 0 and i == 0)
            for hh in range(2):
                h = 2 * j + hh
                kv = j
                rs_ = slice(kv * 64, (kv + 1) * 64)
                ST = P.psb()
                for s_ in range(2):
                    kcols = slice(i * 128 + s_ * 128, i * 128 + s_ * 128 + 128)
                    P.mm(ST[:, s_ * 128:(s_ + 1) * 128], khat[rs_, kcols], qh[rs_, h % 2, bs], start=True, stop=False)
                    bb = self.biasb[:, 8, :] if (first and s_ == 0) else self.biasb[:, h * 2 + s_, :]
                    P.mm(ST[:, s_ * 128:(s_ + 1) * 128], self.identb, bb, start=False, stop=True)
                PT = self.r_att[:, n_att % 2, :]
                n_att += 1
                P.act(PT, ST[:, 0:256], AF.Exp, scale=0.125)
                yield
                for s_ in range(2):
                    vv = vp[:, i + s_, :].rearrange("p (v n) -> p v n", v=4)[:, 2 * kv + hh, :]
                    st_ = (hh == 0 and s_ == 0)
                    sp_ = (hh == 1 and s_ == 1)
                    P.mm(o_all[:, bs], vv, PT[:, s_ * 128:(s_ + 1) * 128], start=st_, stop=sp_)
                    P.mm(den[:, bs], self.onespad[:, hh, :], PT[:, s_ * 128:(s_ + 1) * 128], start=st_, stop=sp_)
            self._gates_some(l, 1)
            yield
        P.ts(a_, den, dc[:, 4 + j:5 + j], None, ALU.add)
        P.op("dve", lambda e, a_=a_, b_=b_: e.reciprocal(b_, a_), [b_], [a_])
        P.tt(yT[:, j, :], o_all, b_, ALU.mult)
        yield
    P.copy(khat[:, 0:128], khat[:, G:G + 128], eng="pool")
    P.copy(self.vcar[:, l, :], vp[:, NB, :], eng="pool")
    yield


def _rglru(self, l, g, j, temps, lgraw, xcb):
    P = self.P
    dc = self.dcols[:, l, :]
    yT = self.yT[3]
    a_, b_, c_, d_, e_ = temps
    wt = self._fm_load(l, 15 + j, 1)
    P.copy(self.lxh[:, j, 0:3], self.lxc[:, l * 2 + j, 0:3], eng="pool")
    P.copy(self.lxh[:, j, 3:3 + G], self._proj(wt, 0))
    wt = self._fm_load(l, 17 + j, 1)
    P.copy(lgraw, self._proj(wt, 0))
    yield
    lx = self.lxh[:, j, :]
    cw = self.pc(("conv_w", l), j * 4, 4)
    xc = a_
    P.ts(xc, lx[:, 0:G], cw[:, 0:1], self.pc(("conv_b", l), j), ALU.mult, ALU.add)
    for jt in range(1, 4):
        P.stt(xc, lx[:, jt:jt + G], cw[:, jt:jt + 1], xc, ALU.mult, ALU.add)
    P.copy(xcb, xc, eng="pool")
    yield
    pr = P.psb()
    pi_ = P.psb()
    P.mm(pr, self.lruw[:, l * 4 + j, :], xcb)
    P.mm(pi_, self.lruw[:, l * 4 + 2 + j, :], xcb)
    P.act(b_, pr, AF.Sigmoid, bias=self.pc(("lru_ba", l), j))
    P.act(c_, pi_, AF.Sigmoid, bias=self.pc(("lru_bx", l), j))
    yield
    P.tt(c_, c_, xc, ALU.mult)
    P.act(d_, b_, AF.Exp, scale=dc[:, 6 + j:7 + j])
    P.act(e_, b_, AF.Exp, scale=dc[:, 8 + j:9 + j])
    P.act(e_, e_, AF.Ln, scale=-1.0, bias=1.0)
    P.act(e_, e_, AF.Exp, scale=0.5)
    yield
    P.tt(c_, c_, e_, ALU.mult)
    hcol = self.hst[:, l, j:j + 1]
    P.scan(b_, d_, c_, hcol, ALU.mult, ALU.add)
    P.copy(hcol, b_[:, G - 1:G], eng="pool")
    yield
    xg = lgraw
    P.act(d_, xg, AF.Square)
    P.ts(d_, d_, 0.044715, 1.0, ALU.mult, ALU.add)
    P.tt(d_, d_, xg, ALU.mult)
    P.act(d_, d_, AF.Tanh, scale=0.7978845608028654)
    yield
    P.stt(d_, d_, 1.0, xg, ALU.add, ALU.mult)
    P.stt(yT[:, j, :], b_, 0.5, d_, ALU.mult, ALU.mult)
    P.copy(self.lxc[:, l * 2 + j, 0:3], lx[:, G:G + 3], eng="pool")
    self._gates_some(l, 2)
    yield


def _merge(self, l, g):
    P = self.P
    wbv = self.wbf[("wbr", l)].rearrange("(a p) n -> p a n", p=128)
    wov = self.wbf[("wout", l)].rearrange("(c p) n -> p c n", p=128)
    wts = []
    for hf in range(2):
        wt = self.wslot().rearrange("p (a n) -> p a n", a=4)
        self.wload(wt, wbv[:, hf * 4:(hf + 1) * 4, :])
        wts.append(wt)
    wo = self.wslot().rearrange("p (c n) -> p c n", c=NCH)
    self.wload(wo, wov[:, :, 0:512])
    pss = [P.psl(k_) for k_ in range(4)]
    accs = [[self.m_a, self.m_b, self.m_c, self.m_d], [self.r_a, self.r_b, self.r_c, self.r_d],
            [self.m_q[:, 0, :], self.m_q[:, 1, :], self.m_z[:, 0, :], self.m_z[:, 1, :]]]

    def stage1(cch):
        acc = accs[cch % 3]
        for n in range(4):
            wt = wts[n // 2]
            ps = P.psb()
            for kc in range(2):
                P.mm(ps, wt[:, (n % 2) * 2 + kc, cch * 128:(cch + 1) * 128], self.yT[n][:, kc, :], start=(kc == 0), stop=(kc == 1))
            P.tt(acc[n], ps, self.gatesb[:, n * 8 + cch, :], ALU.mult)

    def stage2(cch):
        acc = accs[cch % 3]
        P.tt(acc[0], acc[0], acc[1], ALU.add, eng="pool")
        P.tt(acc[2], acc[2], acc[3], ALU.add, eng="pool")
        P.tt(self.merged[:, cch, :], acc[0], acc[2], ALU.add, eng="pool")

    stage1(0)
    stage1(1)
    for cch in range(NCH):
        stage2(cch)
        if cch + 2 < NCH:
            stage1(cch + 2)
        for m in range(4):
            P.mm(pss[m], wo[:, cch, m * 128:(m + 1) * 128], self.merged[:, cch, :], start=(cch == 0), stop=(cch == NCH - 1))
    for m in range(4):
        P.tt(self.xT[:, m, :], pss[m], self.xT[:, m, :], ALU.add)
    wo = self.wslot().rearrange("p (c n) -> p c n", c=NCH)
    self.wload(wo, wov[:, :, 512:1024])
    for m in range(4):
        ps = P.psb()
        for cc in range(NCH):
            P.mm(ps, wo[:, cc, m * 128:(m + 1) * 128], self.merged[:, cc, :], start=(cc == 0), stop=(cc == NCH - 1))
        P.tt(self.xT[:, 4 + m, :], ps, self.xT[:, 4 + m, :], ALU.add)


Builder.mixer_setup = _mixer_setup
Builder._gates_some = _gates_some
Builder._fm_load = _fm_load
Builder._proj = _proj
Builder.mixer = _mixer
Builder._rs = _rs
Builder.hgrn = _hgrn
Builder.retention = _retention
Builder.swa = _swa
Builder.rglru = _rglru
Builder.merge = _merge


def host_consts(inp):
    ct, cb = const_table()
    lw = np.zeros((NL, 4, 128, 128), np.float32)
    for l in range(NL):
        for j in range(2):
            for k, nm in ((0, "lru_wa"), (2, "lru_wx")):
                w = np.asarray(inp[nm][l])
                lw[l, k + j, 0:64, 0:64] = w[2 * j]
                lw[l, k + j, 64:128, 64:128] = w[2 * j + 1]
    return {"ctab": ct, "cbf": cb, "lruw": lw}


def kernel(**inputs):
    x = np.asarray(inputs["x"])
    Bn, T, _ = x.shape
    b = Builder(T)
    nc = b.build()
    consts = host_consts(inputs)
    in_maps = []
    for i in range(Bn):
        m = host_inputs(inputs, i)
        m.update(consts)
        in_maps.append(m)
    res = run_bass_kernel_spmd(nc, in_maps, core_ids=list(range(Bn)))
    out = np.stack([np.asarray(r["outT"]).T for r in res.results], axis=0)
    return np.ascontiguousarray(out.astype(np.float32))
```

```python
import numpy as np
import concourse.bass as bass
import concourse.mybir as mybir
from concourse.bass_utils import run_bass_kernel_spmd

F32 = mybir.dt.float32
BF16 = mybir.dt.bfloat16
AF = mybir.ActivationFunctionType
ALU = mybir.AluOpType

D = 1024
DFF = 2816
NL = 2
G = 512
NCH = 8
NHC = 22
EPS = 1e-6
ENGS = ("pe", "act", "dve", "pool", "sp")


class _Op:
    __slots__ = ("eng", "fn", "deps", "signal", "sigval", "dma_key", "dma_val")

    def __init__(self, eng, fn):
        self.eng = eng
        self.fn = fn
        self.deps = []
        self.signal = False
        self.sigval = 0
        self.dma_key = None
        self.dma_val = 0


class Sched:
    def __init__(self):
        self.ops = {e: [] for e in ENGS}
        self.last_w = {}
        self.readers = {}
        self.dma_cnt = {}
        self.batch_keys = set()

    def add(self, eng, fn, reads=(), writes=(), dma_key=None, batch=False):
        op = _Op(eng, fn)
        deps = []
        for r in reads:
            t = self.last_w.get(r)
            if t is not None:
                deps.append(t)
        for w in writes:
            t = self.last_w.get(w)
            if t is not None:
                deps.append(t)
            deps.extend(self.readers.get(w, ()))
        seen = set()
        for d in deps:
            if id(d) in seen or d is op:
                continue
            seen.add(id(d))
            if d.dma_key is None and d.eng == eng and eng == "pe":
                continue
            op.deps.append(d)
            if d.dma_key is None:
                d.signal = True
        if dma_key is not None:
            op.dma_key = dma_key
            self.dma_cnt[dma_key] = self.dma_cnt.get(dma_key, 0) + 16
            op.dma_val = self.dma_cnt[dma_key]
            if batch:
                self.batch_keys.add(dma_key)
        for r in reads:
            self.readers.setdefault(r, []).append(op)
        for w in writes:
            self.last_w[w] = op
            self.readers[w] = []
        self.ops[eng].append(op)
        return op

    def emit(self, nc, final_waits=()):
        for e in ENGS:
            n = 0
            for op in self.ops[e]:
                if op.dma_key is None and op.signal:
                    n += 1
                    op.sigval = n
        import contextlib
        with contextlib.ExitStack() as st:
            esem = {e: st.enter_context(nc.semaphore("s_" + e)) for e in ENGS}
            dsem = {k: st.enter_context(nc.semaphore("d_%s" % (str(k).replace(" ", "").replace("'", "").replace(",", "_").replace("(", "").replace(")", ""))))
                    for k in self.dma_cnt}
            block = st.enter_context(nc.Block())
            sched = self

            def run(e):
                def body(engine):
                    waited = {}
                    for op in sched.ops[e]:
                        for d in op.deps:
                            if d.dma_key is not None:
                                sem = dsem[d.dma_key]
                                val = sched.dma_cnt[d.dma_key] if d.dma_key in sched.batch_keys else d.dma_val
                                key = ("d", d.dma_key)
                            else:
                                sem = esem[d.eng]
                                val = d.sigval
                                key = ("e", d.eng)
                            if waited.get(key, 0) >= val:
                                continue
                            waited[key] = val
                            engine.wait_ge(sem, val)
                        ins = op.fn(engine)
                        if op.dma_key is not None:
                            ins.then_inc(dsem[op.dma_key], 16)
                        elif op.signal:
                            ins.then_inc(esem[e], 1)
                    if e == "sp":
                        for k in final_waits:
                            engine.wait_ge(dsem[k], sched.dma_cnt[k])
                return body

            block.tensor(run("pe"))
            block.scalar(run("act"))
            block.vector(run("dve"))
            block.gpsimd(run("pool"))
            block.sync(run("sp"))


CELL = 256


def _esz(dt):
    return 4 if dt == F32 else 2


def _cells(ap):
    name = ap.tensor.name
    if name in ("xT", "outT"):
        return [(name, ap.offset)]
    if name not in ("SB", "PS"):
        return [name]
    es = _esz(ap.dtype)
    pat = ap.ap
    pstep = pat[0][0]
    first = ap.offset % pstep if pstep else ap.offset
    last = first
    for st, cnt in pat[1:]:
        last += st * (cnt - 1)
    return [(name, c) for c in range(first * es // CELL, (last * es + es - 1) // CELL + 1)]


class Prog:
    def __init__(self, nc, sb_bytes):
        self.nc = nc
        self.S = Sched()
        self.SB = nc.alloc_sbuf_tensor("SB", [128, sb_bytes // 4], F32).ap()
        self.PS = nc.alloc_psum_tensor("PS", [128, 4096], F32).ap()
        self.sb_bytes = sb_bytes
        self.off = 0
        self.bank = 0

    def alloc(self, nbytes, dt, shape=None):
        nbytes = (nbytes + CELL - 1) // CELL * CELL
        o = self.off
        self.off += nbytes
        assert self.off <= self.sb_bytes, ("SBUF overflow", self.off)
        v = self.SB[:, o // 4:(o + nbytes) // 4]
        if dt != F32:
            v = v.bitcast(dt)
        return v

    def tile(self, dt, *shape):
        n = 1
        for s_ in shape:
            n *= s_
        v = self.alloc(n * _esz(dt), dt)[:, 0:n]
        if len(shape) == 2:
            return v.rearrange("p (a b) -> p a b", a=shape[0])
        if len(shape) == 3:
            return v.rearrange("p (a b c) -> p a b c", a=shape[0], b=shape[1])
        if len(shape) == 4:
            return v.rearrange("p (a b c d) -> p a b c d", a=shape[0], b=shape[1], c=shape[2])
        return v

    nrot = 4

    def psb(self, n=1):
        if self.bank + n > self.nrot:
            self.bank = 0
        b = self.bank
        self.bank = (self.bank + n) % self.nrot
        return self.PS[:, b * 512:(b + n) * 512]

    def psl(self, k):
        return self.PS[:, (4 + k) * 512:(5 + k) * 512]

    def op(self, eng, fn, outs, ins, dma_key=None, batch=False):
        r = []
        for a in ins:
            r.extend(_cells(a))
        w = []
        for a in outs:
            w.extend(_cells(a))
        return self.S.add(eng, fn, reads=r, writes=w, dma_key=dma_key, batch=batch)

    def mm(self, out, lhsT, rhs, start=True, stop=True):
        self.op("pe", lambda e: e.matmul(out, lhsT, rhs, start=start, stop=stop), [out], [lhsT, rhs] + ([] if start else [out]))

    def tr(self, out, in_, ident):
        self.op("pe", lambda e: e.transpose(out, in_, ident), [out], [in_, ident])

    def act(self, out, in_, func, bias=None, scale=None, eng="act"):
        ins = [in_]
        kw = {}
        if bias is not None:
            kw["bias"] = bias
            if not isinstance(bias, (int, float)):
                ins.append(bias)
        if scale is not None:
            kw["scale"] = scale
            if not isinstance(scale, (int, float)):
                ins.append(scale)
        self.op(eng, lambda e: e.activation(out, in_, func, **kw), [out], ins)

    def tt(self, out, a, b, op, eng="dve"):
        self.op(eng, lambda e: e.tensor_tensor(out, a, b, op), [out], [a, b])

    def ts(self, out, a, s1, s2, op0, op1=None, eng="dve"):
        ins = [a] + [s for s in (s1, s2) if s is not None and not isinstance(s, (int, float))]
        if op1 is None:
            self.op(eng, lambda e: e.tensor_scalar(out, a, s1, None, op0), [out], ins)
        else:
            self.op(eng, lambda e: e.tensor_scalar(out, a, s1, s2, op0, op1), [out], ins)

    def stt(self, out, a, sc, b, op0, op1, eng="dve"):
        ins = [a, b] + ([] if isinstance(sc, (int, float)) else [sc])
        self.op(eng, lambda e: e.scalar_tensor_tensor(out, a, sc, b, op0, op1), [out], ins)

    def copy(self, out, a, eng="dve"):
        self.op(eng, lambda e: e.tensor_copy(out, a), [out], [a])

    def memset(self, out, val, eng="pool"):
        self.op(eng, lambda e: e.memset(out, val), [out], [])

    def scan(self, out, d0, d1, init, op0, op1, eng="dve"):
        ins = [d0, d1] + ([] if isinstance(init, (int, float)) else [init])
        self.op(eng, lambda e: e.tensor_tensor_scan(out, d0, d1, init, op0, op1), [out], ins)

    def dma(self, eng, out, in_, key, batch=False):
        self.op(eng, lambda e: e.dma_start(out=out, in_=in_), [out], [in_], dma_key=key, batch=batch)


def _pc_layout():
    names = {}
    n = 0

    def add(key, cnt):
        nonlocal n
        names[key] = n
        n += cnt
    for l in range(NL):
        add(("ffn1_norm", l), 8)
        add(("mix_norm", l), 8)
        add(("ffn2_norm", l), 8)
        add(("gate_bias", l), 32)
        add(("lb_logit", l), 2)
        add(("hg_outw", l), 2)
        add(("gn_w", l), 2)
        add(("gn_b", l), 2)
        add(("qn_w", l), 1)
        add(("kn_w", l), 1)
        add(("sink", l), 2)
        add(("conv_w", l), 8)
        add(("conv_b", l), 2)
        add(("lru_ba", l), 2)
        add(("lru_bx", l), 2)
        add(("lru_lam", l), 2)
    return names, n


PC, NPC = _pc_layout()


def _fm(v):
    return np.ascontiguousarray(np.asarray(v, np.float32).reshape(-1, 128).T)


class Builder:
    def __init__(self, T, phases=("ffn1", "mix", "ffn2"), n_layers=NL, dbg=None):
        self.T = T
        self.NG = T // G
        self.phases = phases
        self.n_layers = n_layers
        self.dbg = dbg
        nc = self.nc = bass.Bass("TRN2", target_bir_lowering=False)
        self.P = P = Prog(nc, 207 * 1024)
        dt = nc.dram_tensor
        self.x_in = dt("xT", [D, T], F32, kind="ExternalInput").ap()
        self.out = dt("outT", [D, T], F32, kind="ExternalOutput").ap()
        self.pcols_d = dt("pcols", [128, NPC], F32, kind="ExternalInput").ap()
        self.w32 = {}
        self.wbf = {}
        for nm, shp in (("wg1", [D, DFF]), ("wu1", [D, DFF]), ("wd1", [DFF, D]),
                        ("win", [D, WIN_COLS]), ("wbr", [1024, D]), ("wout", [D, D]),
                        ("wg2", [D, DFF]), ("wu2", [D, DFF]), ("wd2", [DFF, D])):
            for l in range(n_layers):
                self.w32[(nm, l)] = dt("%s_%d" % (nm, l), shp, F32, kind="ExternalInput").ap()
                self.wbf[(nm, l)] = dt("b_%s_%d" % (nm, l), shp, BF16, kind="Internal").ap()
        self.xT = P.tile(F32, NCH, G)
        self.hT = P.tile(BF16, NCH, G)
        self.pcols = P.tile(F32, 1, NPC)[:, 0, :]
        self.big = P.alloc(32768, BF16)
        self.sqb = self.big[:, 0:NCH * G].rearrange("p (c n) -> p c n", c=NCH)
        self.wslots = [P.alloc(8192, BF16) for _ in range(3)]
        self.wslot_i = 0
        self.m_a, self.m_b, self.m_c, self.m_d, self.m_e = [P.tile(F32, 1, G)[:, 0, :] for _ in range(5)]
        self.rstd = self.m_e
        self.onesD = P.tile(BF16, 1, 128)[:, 0, :]
        self.cst32 = P.tile(F32, 1, 128)[:, 0, :]

    def pc(self, key, j=0, n=1):
        o = PC[key] + j
        return self.pcols[:, o:o + n]

    def wslot(self):
        s = self.wslots[self.wslot_i]
        self.wslot_i = (self.wslot_i + 1) % len(self.wslots)
        return s

    def wload(self, dst, src):
        self.P.dma("sp", dst, src, key=("w", id(dst.tensor), dst.offset))

    def convert_all(self):
        P = self.P
        order = ("wg1", "wu1", "wd1", "win", "wbr", "wout", "wg2", "wu2", "wd2")
        for l in range(self.n_layers):
            for nm in order:
                P.dma("pool", self.wbf[(nm, l)], self.w32[(nm, l)], key=("cv", nm, l))

    def consts(self):
        P = self.P
        P.dma("act", self.pcols, self.pcols_d, key="c0")
        P.memset(self.cst32, 1.0 / D)
        P.copy(self.onesD, self.cst32, eng="pool")

    def rmsnorm(self, wkey, l):
        P = self.P
        ps = P.psb()
        for c in range(NCH):
            P.act(self.sqb[:, c, :], self.xT[:, c, :], AF.Square)
        for c in range(NCH):
            P.mm(ps, self.onesD, self.sqb[:, c, :], start=(c == 0), stop=(c == NCH - 1))
        P.act(self.rstd, ps, AF.Ln, bias=self.eps_col)
        P.act(self.rstd, self.rstd, AF.Exp, scale=-0.5)
        for c in range(NCH):
            P.stt(self.hT[:, c, :], self.xT[:, c, :], self.pc((wkey, l), c), self.rstd, ALU.mult, ALU.mult)

    def ffn(self, l, which):
        P = self.P
        wg, wu, wd = self.wbf[("wg" + which, l)], self.wbf[("wu" + which, l)], self.wbf[("wd" + which, l)]
        self.rmsnorm("ffn%s_norm" % which, l)
        actT = self.big[:, 0:NHC * G].rearrange("p (j n) -> p j n", j=NHC)
        wgv = wg.rearrange("(c p) n -> p c n", p=128)
        wuv = wu.rearrange("(c p) n -> p c n", p=128)
        P.nrot = 8
        P.bank = 0
        for jp in range(NHC // 2):
            slot = self.wslot()
            wt = slot.rearrange("p (u c n) -> p u c n", u=2, c=NCH)
            self.wload(wt[:, 0], wgv[:, :, jp * 256:(jp + 1) * 256])
            self.wload(wt[:, 1], wuv[:, :, jp * 256:(jp + 1) * 256])
            for jj in range(2):
                j = jp * 2 + jj
                pg = P.psb()
                pu = P.psb()
                for c in range(NCH):
                    P.mm(pg, wt[:, 0, c, jj * 128:(jj + 1) * 128], self.hT[:, c, :], start=(c == 0), stop=(c == NCH - 1))
                for c in range(NCH):
                    P.mm(pu, wt[:, 1, c, jj * 128:(jj + 1) * 128], self.hT[:, c, :], start=(c == 0), stop=(c == NCH - 1))
                sg = self.tmps[j % 3]
                P.act(sg, pg, AF.Silu)
                P.tt(actT[:, j, :], pu, sg, ALU.mult)
        P.nrot = 4
        P.bank = 0
        wdv = wd.rearrange("(j p) n -> p j n", p=128)
        for half in range(2):
            pss = [P.psl(k_) for k_ in range(4)]
            j0 = 0
            for nj in (8, 8, 6):
                slot = self.wslot()
                wt = slot.rearrange("p (j n) -> p j n", j=8)
                self.wload(wt[:, 0:nj], wdv[:, j0:j0 + nj, half * 512:(half + 1) * 512])
                for jj in range(nj):
                    j = j0 + jj
                    for m in range(4):
                        P.mm(pss[m], wt[:, jj, m * 128:(m + 1) * 128], actT[:, j, :], start=(j == 0), stop=(j == NHC - 1))
                j0 += nj
            for m in range(4):
                c = half * 4 + m
                P.stt(self.xT[:, c, :], pss[m], 0.5, self.xT[:, c, :], ALU.mult, ALU.add)

    def build(self):
        P = self.P
        self.eps_col = P.tile(F32, 1, 1)[:, 0, :]
        self.tmps = [self.m_a, self.m_b, self.m_c]
        P.memset(self.eps_col, EPS)
        self.consts()
        self.convert_all()
        if "mix" in self.phases:
            self.mixer_setup()
        xv = self.x_in.rearrange("(c p) t -> p c t", p=128)
        ov = self.out.rearrange("(c p) t -> p c t", p=128)
        for g in range(self.NG):
            for cx in range(NCH):
                P.dma("pool", self.xT[:, cx, :], xv[:, cx, g * G:(g + 1) * G], key=("xin", cx))
            for l in range(self.n_layers):
                if "ffn1" in self.phases:
                    self.ffn(l, "1")
                if "mix" in self.phases:
                    self.mixer(l, g)
                if "ffn2" in self.phases:
                    self.ffn(l, "2")
            for cx in range(NCH):
                P.dma("pool", ov[:, cx, g * G:(g + 1) * G], self.xT[:, cx, :], key=("xout", cx))
        P.S.emit(self.nc, final_waits=[("xout", cx) for cx in range(NCH)])
        return self.nc


WIN_COLS = 51 * 128 + 896


def _win_perm():
    o = {}
    names = ("hq", "hf", "hi", "hg", "rq", "rk", "rv", "rg", "aq", "ak", "av", "lx", "lg", "gate")
    sizes = (256, 256, 256, 256, 256, 256, 256, 256, 256, 128, 128, 256, 256, 4096)
    s = 0
    for n_, z in zip(names, sizes):
        o[n_] = s
        s += z
    r = lambda n_, a=0, b=None: list(range(o[n_] + a, o[n_] + (b if b is not None else dict(zip(names, sizes))[n_])))
    cols = []
    cols += r("gate")
    cols += r("hq") + r("hf") + r("hg")
    cols += r("rq") + r("rk") + r("rg")
    cols += r("aq", 0, 64) + r("aq", 128, 192) + r("aq", 64, 128) + r("aq", 192, 256)
    cols += r("ak")
    cols += r("lx") + r("lg")
    cols += r("rk") + r("rv") + r("hi") + r("av")
    return np.array(cols, np.int64)


def host_inputs(inp, b, T=None):
    x = np.asarray(inp["x"])[b]
    if T is not None:
        x = x[:T]
    m = {"xT": np.ascontiguousarray(x.T)}
    pc = np.zeros((128, NPC), np.float32)
    perm = _win_perm()
    for l in range(NL):
        def put(key, arr):
            a = _fm(arr)
            pc[:, PC[(key, l)]:PC[(key, l)] + a.shape[1]] = a
        put("ffn1_norm", inp["ffn1_norm"][l])
        put("mix_norm", inp["mix_norm"][l])
        put("ffn2_norm", inp["ffn2_norm"][l])
        put("gate_bias", np.asarray(inp["gate_bias"][l]).reshape(-1))
        put("lb_logit", inp["hgrn_lb_logits"][l])
        put("hg_outw", inp["hgrn_out_norm"][l])
        put("gn_w", inp["ret_gn_w"][l])
        put("gn_b", inp["ret_gn_b"][l])
        put("qn_w", np.tile(np.asarray(inp["attn_q_norm"][l]), 2))
        put("kn_w", np.tile(np.asarray(inp["attn_k_norm"][l]), 2))
        put("sink", np.repeat(np.asarray(inp["attn_sinks"][l]), 64))
        cw = np.asarray(inp["lru_conv_w"][l])
        put("conv_w", np.concatenate([cw[:, 0:128].reshape(-1), cw[:, 128:256].reshape(-1)]).reshape(8, 128).reshape(-1))
        put("conv_b", inp["lru_conv_b"][l])
        put("lru_ba", inp["lru_ba"][l])
        put("lru_bx", inp["lru_bx"][l])
        put("lru_lam", inp["lru_lambda"][l])
        m["wg1_%d" % l] = np.asarray(inp["ffn1_wg"][l])
        m["wu1_%d" % l] = np.asarray(inp["ffn1_wu"][l])
        m["wd1_%d" % l] = np.asarray(inp["ffn1_wd"][l])
        m["wg2_%d" % l] = np.asarray(inp["ffn2_wg"][l])
        m["wu2_%d" % l] = np.asarray(inp["ffn2_wu"][l])
        m["wd2_%d" % l] = np.asarray(inp["ffn2_wd"][l])
        m["win_%d" % l] = np.ascontiguousarray(np.asarray(inp["w_in"][l])[:, perm])
        m["wbr_%d" % l] = np.ascontiguousarray(np.asarray(inp["w_branch"][l]).reshape(1024, D))
        m["wout_%d" % l] = np.asarray(inp["w_out"][l])
    m["pcols"] = pc
    return m


def _ct_layout():
    names = {}
    n = 0

    def add(key, cnt):
        nonlocal n
        names[key] = (n, cnt)
        n += cnt
    add("ident", 128)
    add("blk64", 128)
    add("bdmask", 128)
    add("reset", G)
    add("cmask8", 8)
    add("hmaskT2", 256)
    for p in range(2):
        add(("decayT", p), 256)
        add(("qdecay", p), 128)
    add("kdecay", 256)
    add("g128", 2)
    return names, n


CT, NCT = _ct_layout()
NCB = 128 + 128 + 9 * 128 + 256


def const_table():
    t = np.zeros((128, NCT), np.float64)

    def put(key, arr):
        o, c = CT[key]
        t[:, o:o + c] = arr
    idx = np.arange(128)
    put("ident", np.eye(128))
    blk = (idx[:, None] // 64 == idx[None, :] // 64).astype(np.float64)
    put("blk64", blk / 64.0)
    put("bdmask", blk)
    tt = np.arange(G)
    put("reset", np.broadcast_to((tt % 16 != 0).astype(np.float64), (128, G)))
    put("cmask8", (idx[:, None] // 16 == np.arange(8)[None, :]).astype(np.float64))
    hm = ((idx[:, None] // 16 == idx[None, :] // 16) & (idx[:, None] <= idx[None, :])).astype(np.float64)
    put("hmaskT2", np.concatenate([hm, hm], 1))
    lg = np.log1p(-np.exp2(-5.0 - np.arange(4)))
    rel = idx[None, :] - idx[:, None]
    kd = np.zeros((128, 256))
    g128 = np.zeros((128, 2))
    for p in range(2):
        dT = []
        qd = np.zeros((128, 128))
        for hh in range(2):
            h = 2 * p + hh
            dT.append(np.where(rel >= 0, np.exp(lg[h] * np.maximum(rel, 0)), 0.0) / 8.0)
            qd[hh * 64:(hh + 1) * 64, :] = np.exp(lg[h] * (idx[None, :] + 1.0)) / 8.0
            kd[:, h * 64:(h + 1) * 64] = np.exp(lg[h] * (127.0 - idx))[:, None]
            g128[hh * 64:(hh + 1) * 64, p] = np.exp(lg[h] * 128.0)
        put(("decayT", p), np.concatenate(dT, 1))
        put(("qdecay", p), qd)
    put("kdecay", kd)
    put("g128", g128)
    tb = np.zeros((128, NCB), np.float64)
    tb[:, 0:128] = np.eye(128)
    tb[:, 128:256] = blk / 64.0
    slopes = np.exp2(-8.0 * np.arange(1, 5) / 4.0)
    NEG = -240000.0
    kk = idx[:, None]
    qq = idx[None, :]
    for h in range(4):
        prev = np.where(kk > qq, -slopes[h] * (qq + 128 - kk) * 8.0, NEG)
        cur = np.where(kk <= qq, -slopes[h] * (qq - kk) * 8.0, NEG)
        tb[:, 256 + (2 * h) * 128:256 + (2 * h + 1) * 128] = prev
        tb[:, 256 + (2 * h + 1) * 128:256 + (2 * h + 2) * 128] = cur
    tb[:, 256 + 8 * 128:256 + 9 * 128] = NEG
    op = np.zeros((128, 256))
    op[:, 0:64] = 1.0
    op[:, 128 + 64:256] = 1.0
    tb[:, 256 + 9 * 128:] = op
    return t.astype(np.float32), tb.astype(np.float32)


NB = G // 128
import os
SKIP = os.environ.get('SKIP', '')
STOP = int(os.environ.get('STOP', '99'))
FMC = 19


def _mixer_setup(self):
    P = self.P
    nc = self.nc
    self.ctab_d = nc.dram_tensor("ctab", [128, NCT], F32, kind="ExternalInput").ap()
    self.lruw_d = nc.dram_tensor("lruw", [NL, 4, 128, 128], F32, kind="ExternalInput").ap()
    self.ct = P.tile(F32, 1, NCT)[:, 0, :]
    P.dma("act", self.ct, self.ctab_d, key="c3")
    c = lambda key: self.ct[:, CT[key][0]:CT[key][0] + CT[key][1]]
    self.c = c
    self.cbf_d = nc.dram_tensor("cbf", [128, NCB], F32, kind="ExternalInput").ap()
    cb = P.tile(BF16, 1, NCB)[:, 0, :]
    P.dma("pool", cb, self.cbf_d, key="c1")
    self.identb = cb[:, 0:128]
    self.blk64b = cb[:, 128:256]
    self.biasb = cb[:, 256:256 + 9 * 128].rearrange("p (a n) -> p a n", a=9)
    self.onespad = cb[:, 256 + 9 * 128:].rearrange("p (a n) -> p a n", a=2)
    self.lruw = P.tile(BF16, NL * 4, 128)
    P.dma("pool", self.lruw, self.lruw_d.rearrange("l j p n -> p (l j) n"), key="c2")
    self.dcols = P.tile(F32, NL, 16)
    for l in range(self.n_layers):
        dc = self.dcols[:, l, :]
        if l == 0:
            P.memset(dc[:, 0:2], 0.0)
        else:
            P.tt(dc[:, 10:12], self.pc(("lb_logit", 1), 0, 2), self.pc(("lb_logit", 0), 0, 2), ALU.subtract)
            P.act(dc[:, 0:2], dc[:, 10:12], AF.Sigmoid)
        P.ts(dc[:, 2:4], dc[:, 0:2], -1.0, 1.0, ALU.mult, ALU.add)
        P.act(dc[:, 4:6], self.pc(("sink", l), 0, 2), AF.Exp)
        P.act(dc[:, 12:14], self.pc(("lru_lam", l), 0, 2), AF.Exp, scale=-1.0)
        P.act(dc[:, 12:14], dc[:, 12:14], AF.Ln, bias=1.0)
        P.ts(dc[:, 6:8], dc[:, 12:14], -8.0, None, ALU.mult)
        P.ts(dc[:, 8:10], dc[:, 12:14], -16.0, None, ALU.mult)
    L = self.n_layers
    self.Sh = P.tile(F32, 9, 128)
    self.Sc = P.tile(F32, L * 2, 128)
    self.Sr = P.tile(F32, L * 2, 128)
    self.khat = P.tile(BF16, L, 128 + G)
    self.vpad = P.tile(BF16, NB + 1, 4 * 128)
    self.vcar = P.tile(BF16, L, 4 * 128)
    self.lxh = P.tile(F32, 2, G + 4)
    self.lxc = P.tile(F32, L * 2, 4)
    self.hst = P.tile(F32, L, 2)
    for t_ in (self.Sh, self.Sc, self.Sr, self.khat, self.vpad, self.vcar, self.lxh, self.lxc, self.hst):
        P.memset(t_, 0.0)
    self.gatesb = self.big[:, 0:32 * G].rearrange("p (j n) -> p j n", j=32)
    f32t = lambda *s: P.tile(F32, *s)
    bft = lambda *s: P.tile(BF16, *s)
    self.m_q = f32t(2, G)
    self.m_z = f32t(2, G)
    self.m_sg = f32t(2, G)
    self.m_qd = f32t(2, G)
    self.m_kpp = self.m_e
    self.m_qt = bft(2, G)
    self.m_kt = bft(2, G)
    self.m_dcol = f32t(2, 32)
    self.m_km8 = bft(8, 128)
    self.m_dSm = f32t(8, 128)
    self.m_att = bft(2, 256)
    self.m_tok = bft(NB, 256)
    self.m_vtok = bft(NB, 256)
    self.m_pad = bft(NB, 4 * 128)
    self.m_tmp128 = f32t(1, 128)[:, 0, :]
    self.m_lg = self.m_q
    self.hi_tok = bft(NB, 256)
    self.hi_pad = bft(NB, 4 * 128)
    self.gslots = [P.alloc(4096, BF16) for _ in range(2)]
    self.r_a, self.r_b, self.r_c, self.r_d = [f32t(1, G)[:, 0, :] for _ in range(4)]
    self.r_qd = f32t(2, G)
    self.r_qt = bft(2, G)
    self.r_kt = bft(2, G)
    self.r_sg = bft(2, G)
    self.r_att = bft(2, 256)
    P.memset(self.m_pad, 0.0)
    P.memset(self.hi_pad, 0.0)
    self.yT = [bft(2, G) for _ in range(4)]
    self.merged = self.hT
    self.acc = [self.m_a, self.m_b, self.m_c, self.m_d]


def _gates_some(self, l, k):
    P = self.P
    winv = self.wbf[("win", l)].rearrange("(c p) n -> p c n", p=128)
    while k > 0 and self.g_next < 32:
        j = self.g_next
        if j % 2 == 0:
            wt_ = self.gslots[(j // 2) % 2].rearrange("p (c n) -> p c n", c=NCH)
            self.wload(wt_, winv[:, :, j * 128:(j + 2) * 128])
            self.gate_slot = wt_
        wt = self.gate_slot
        ps = P.psb()
        cc = j % 2
        for c in range(NCH):
            P.mm(ps, wt[:, c, cc * 128:(cc + 1) * 128], self.hT[:, c, :], start=(c == 0), stop=(c == NCH - 1))
        P.copy(self.gatesb[:, j, :], ps)
        self.g_next += 1
        k -= 1


def _fm_load(self, l, m0, n):
    winv = self.wbf[("win", l)].rearrange("(c p) n -> p c n", p=128)
    wt = self.wslot().rearrange("p (c n) -> p c n", c=NCH)
    c0 = (32 + m0) * 128
    self.wload(wt[:, :, 0:n * 128], winv[:, :, c0:c0 + n * 128])
    return wt


def _proj(self, wt, k):
    P = self.P
    ps = P.psb()
    for cc in range(NCH):
        P.mm(ps, wt[:, cc, k * 128:(k + 1) * 128], self.hT[:, cc, :], start=(cc == 0), stop=(cc == NCH - 1))
    return ps


def _mixer(self, l, g):
    P = self.P
    c = self.c
    self.rmsnorm("mix_norm", l)
    winv = self.wbf[("win", l)].rearrange("(c p) n -> p c n", p=128)
    TM0 = 51 * 128
    slot0 = self.wslot().rearrange("p (c n) -> p c n", c=NCH)
    self.wload(slot0, winv[:, :, TM0:TM0 + 512])
    slot1 = self.wslot().rearrange("p (c n) -> p c n", c=NCH)
    self.wload(slot1[:, :, 0:384], winv[:, :, TM0 + 512:TM0 + 896])
    self.wload(slot1[:, :, 384:512], winv[:, :, TM0 + 768:TM0 + 896])
    vp = self.vpad
    P.copy(vp[:, 0, :], self.vcar[:, l, :], eng="pool")
    mpad = self.m_pad
    for i in range(NB):
        p0 = P.psb()
        p1 = P.psb()
        for cc in range(NCH):
            P.mm(p0, self.hT[:, cc, i * 128:(i + 1) * 128], slot0[:, cc, :], start=(cc == 0), stop=(cc == NCH - 1))
        for cc in range(NCH):
            P.mm(p1, self.hT[:, cc, i * 128:(i + 1) * 128], slot1[:, cc, :], start=(cc == 0), stop=(cc == NCH - 1))
        P.tt(self.m_tok[:, i, :], p0[:, 0:256], c("kdecay"), ALU.mult)
        P.copy(self.m_vtok[:, i, :], p0[:, 256:512])
        pad4 = lambda t_: bass.AP(t_.tensor, t_.offset, [list(t_.ap[0]), [256, 2], [192, 2], [1, 64]])
        P.copy(pad4(mpad[:, i, 0:64]), p0[:, 256:512].rearrange("p (a s n) -> p a s n", a=2, s=2))
        P.copy(self.hi_tok[:, i, :], p1[:, 0:256])
        P.copy(pad4(self.hi_pad[:, i, 0:64]), p1[:, 0:256].rearrange("p (a s n) -> p a s n", a=2, s=2))
        P.copy(pad4(vp[:, i + 1, 0:64]), p1[:, 256:384].rearrange("p (k n) -> p k n", k=2).unsqueeze(2).to_broadcast([128, 2, 2, 64]))
    self.g_next = 0

    def chain(*gens):
        for g_ in gens:
            yield from g_
    lru0 = self.rglru(l, g, 0, (self.m_a, self.m_b, self.m_c, self.m_d, self.m_e), self.m_q[:, 0, :], self.m_kt[:, 0, :])
    lru1 = self.rglru(l, g, 1, (self.r_a, self.r_b, self.r_c, self.r_d, self.r_qd[:, 0, :]), self.r_qd[:, 1, :], self.r_kt[:, 1, :])
    streams = [chain(self.hgrn(l, g), lru0), chain(self.retention(l, g), self.swa(l, g), lru1)]
    while streams:
        for s_ in list(streams):
            try:
                next(s_)
            except StopIteration:
                streams.remove(s_)
    self._gates_some(l, 32)
    self.merge(l, g)


def _rs(self, out, ps):
    P = self.P
    P.act(out, ps, AF.Ln, bias=self.eps_col)
    P.act(out, out, AF.Exp, scale=-0.5)


def _hgrn(self, l, g):
    P = self.P
    c = self.c
    dc = self.dcols[:, l, :]
    a_, b_, c_, d_, e_ = self.m_a, self.m_b, self.m_c, self.m_d, self.m_e
    yT = self.yT[0]
    wt = self._fm_load(l, 0, 4)
    for p in range(2):
        P.copy(self.m_q[:, p, :], self._proj(wt, p))
    for p in range(2):
        P.act(self.m_z[:, p, :], self._proj(wt, 2 + p), AF.Sigmoid)
    wt = self._fm_load(l, 4, 2)
    for p in range(2):
        P.act(self.m_sg[:, p, :], self._proj(wt, p), AF.Sigmoid)
    yield
    for p in range(2):
        q = self.m_q[:, p, :]
        f = self.m_z[:, p, :]
        Sh = self.Sh
        P.copy(Sh[:, 0, :], self.Sc[:, l * 2 + p, :], eng="pool")
        P.ts(f, f, dc[:, 2 + p:3 + p], dc[:, p:p + 1], ALU.mult, ALU.add)
        P.ts(a_, f, -1.0, 1.0, ALU.mult, ALU.add)
        P.act(e_, f, AF.Ln)
        P.scan(b_, c("reset"), e_, 0.0, ALU.mult, ALU.add)
        yield
        b3 = b_.rearrange("p (n j) -> p n j", j=16)
        P.tt(c_.rearrange("p (n j) -> p n j", j=16), b3, b3[:, :, 8:9].to_broadcast([128, G // 16, 16]), ALU.subtract)
        P.act(d_, c_, AF.Exp)
        P.tt(self.m_qt[:, p, :], q, d_, ALU.mult)
        yield
        P.act(d_, c_, AF.Exp, scale=-1.0)
        P.tt(self.m_kt[:, p, :], a_, d_, ALU.mult, eng="pool")
        P.act(d_, b_, AF.Exp)
        P.tt(self.m_qd[:, p, :], q, d_, ALU.mult)
        yield
        P.tt(c_.rearrange("p (n j) -> p n j", j=16), b3[:, :, 15:16].to_broadcast([128, G // 16, 16]), b3, ALU.subtract, eng="pool")
        P.act(d_, c_, AF.Exp)
        P.tt(self.m_kpp, a_, d_, ALU.mult, eng="pool")
        P.act(self.m_dcol[:, p, :], b3[:, :, 15], AF.Exp)
        yield
        o_all = P.psl(p)
        for i in range(NB):
            bs = slice(i * 128, (i + 1) * 128)
            kT = P.psb()
            P.tr(kT[:, 0:128], self.m_kpp[:, bs], c("ident"))
            P.tt(self.m_km8, kT[:, 0:128].unsqueeze(1).to_broadcast([128, 8, 128]),
                 c("cmask8").unsqueeze(2).to_broadcast([128, 8, 128]), ALU.mult)
            yield
            dS = P.psb(2)
            for cc in range(8):
                P.mm(dS[:, cc * 128:(cc + 1) * 128], self.m_km8[:, cc, :], self.hi_tok[:, i, p * 128:(p + 1) * 128])
            for hb in range(2):
                P.tt(self.m_dSm[:, hb * 4:(hb + 1) * 4, :], dS[:, hb * 512:(hb + 1) * 512].rearrange("p (a n) -> p a n", a=4),
                     c("bdmask").unsqueeze(1).to_broadcast([128, 4, 128]), ALU.mult)
            yield
            for cc in range(8):
                P.stt(Sh[:, cc + 1, :], Sh[:, cc, :], self.m_dcol[:, p, i * 8 + cc:i * 8 + cc + 1], self.m_dSm[:, cc, :], ALU.mult, ALU.add)
            aT = P.psb(2)
            for hh in range(2):
                rs_ = slice(hh * 64, (hh + 1) * 64)
                P.mm(aT[:, hh * 512:hh * 512 + 128], self.m_kt[rs_, p, bs], self.m_qt[rs_, p, bs])
            att = self.m_att[:, i % 2, :]
            P.tt(att.rearrange("p (a n) -> p a n", a=2), aT.rearrange("p (a n) -> p a n", a=2)[:, :, 0:128],
                 c("hmaskT2").rearrange("p (a n) -> p a n", a=2), ALU.mult)
            yield
            hp = self.hi_pad[:, i, :].rearrange("p (a s n) -> p a s n", a=2, s=2)
            P.mm(o_all[:, bs], hp[:, p, 0, :], att[:, 0:128], start=True, stop=False)
            P.mm(o_all[:, bs], hp[:, p, 1, :], att[:, 128:256], start=False, stop=False)
            for cc in range(8):
                cs = slice(i * 128 + cc * 16, i * 128 + cc * 16 + 16)
                P.mm(o_all[:, cs], Sh[:, cc, :], self.m_qd[:, p, cs], start=False, stop=(cc == 7))
            P.copy(Sh[:, 0, :], Sh[:, 8, :], eng="pool")
            self._gates_some(l, 1)
            yield
        P.copy(self.Sc[:, l * 2 + p, :], Sh[:, 0, :], eng="pool")
        sq = self.m_kt[:, p, :]
        P.act(sq, o_all, AF.Square)
        ms = P.psb()
        P.mm(ms, self.blk64b, sq)
        self._rs(d_, ms)
        yield
        P.stt(c_, o_all, self.pc(("hg_outw", l), p), d_, ALU.mult, ALU.mult)
        P.tt(yT[:, p, :], c_, self.m_sg[:, p, :], ALU.mult)
        yield


def _retention(self, l, g):
    P = self.P
    c = self.c
    yT = self.yT[1]
    a_, b_, c_, d_ = self.r_a, self.r_b, self.r_c, self.r_d
    qt, kt, qd, sg = self.r_qt, self.r_kt, self.r_qd, self.r_sg
    wt = self._fm_load(l, 6, 4)
    for p in range(2):
        ps = self._proj(wt, p)
        P.copy(qt[:, p, :], ps)
        P.tt(qd[:, p, :].rearrange("p (b t) -> p b t", b=NB), ps.rearrange("p (b t) -> p b t", b=NB),
             c(("qdecay", p)).unsqueeze(1).to_broadcast([128, NB, 128]), ALU.mult)
    for p in range(2):
        P.copy(kt[:, p, :], self._proj(wt, 2 + p))
    wt = self._fm_load(l, 10, 2)
    for p in range(2):
        P.act(sg[:, p, :], self._proj(wt, p), AF.Silu)
    yield
    o_alls = [P.psl(2), P.psl(3)]
    for i in range(NB):
        bs = slice(i * 128, (i + 1) * 128)
        for p in range(2):
            S_ = self.Sr[:, l * 2 + p, :]
            aT = P.psb(2)
            for hh in range(2):
                rs_ = slice(hh * 64, (hh + 1) * 64)
                P.mm(aT[:, hh * 512:hh * 512 + 128], kt[rs_, p, bs], qt[rs_, p, bs])
            att = self.r_att[:, (i * 2 + p) % 2, :]
            P.tt(att.rearrange("p (a n) -> p a n", a=2), aT.rearrange("p (a n) -> p a n", a=2)[:, :, 0:128],
                 c(("decayT", p)).rearrange("p (a n) -> p a n", a=2), ALU.mult)
            yield
            vp = self.m_pad[:, i, :].rearrange("p (a s n) -> p a s n", a=2, s=2)
            P.mm(o_alls[p][:, bs], vp[:, p, 0, :], att[:, 0:128], start=True, stop=False)
            P.mm(o_alls[p][:, bs], vp[:, p, 1, :], att[:, 128:256], start=False, stop=False)
            P.mm(o_alls[p][:, bs], S_, qd[:, p, bs], start=False, stop=True)
            dS = P.psb()
            P.mm(dS[:, 0:128], self.m_tok[:, i, p * 128:(p + 1) * 128], self.m_vtok[:, i, p * 128:(p + 1) * 128])
            P.tt(self.m_tmp128, dS[:, 0:128], c("bdmask"), ALU.mult)
            P.stt(S_, S_, c("g128")[:, p:p + 1], self.m_tmp128, ALU.mult, ALU.add)
            yield
        self._gates_some(l, 1)
    for p in range(2):
        o_ps = o_alls[p]
        P.act(a_, o_ps, AF.Square) if False else P.copy(a_, o_ps)
        obf = kt[:, p, :]
        P.copy(obf, o_ps)
        mean = P.psb()
        P.mm(mean, self.blk64b, obf)
        P.tt(b_, a_, mean, ALU.subtract)
        yield
        sq = qt[:, p, :]
        P.act(sq, b_, AF.Square)
        var = P.psb()
        P.mm(var, self.blk64b, sq)
        self._rs(d_, var)
        yield
        P.tt(c_, b_, d_, ALU.mult, eng="pool")
        P.ts(c_, c_, self.pc(("gn_w", l), p), self.pc(("gn_b", l), p), ALU.mult, ALU.add)
        P.tt(yT[:, p, :], c_, sg[:, p, :], ALU.mult, eng="pool")
        yield


def _swa(self, l, g):
    P = self.P
    c = self.c
    dc = self.dcols[:, l, :]
    yT = self.yT[2]
    a_, b_, c_, d_ = self.r_a, self.r_b, self.r_c, self.r_d
    qh = self.r_qt
    khat = self.khat[:, l, :]
    vp = self.vpad
    wt = self._fm_load(l, 12, 3)
    raws = [a_, b_, c_]
    for k_ in range(3):
        P.copy(raws[k_], self._proj(wt, k_))
    yield
    for which in range(3):
        raw = raws[which]
        wcol = self.pc(("qn_w", l)) if which < 2 else self.pc(("kn_w", l))
        dst = qh[:, which, :] if which < 2 else khat[:, 128:128 + G]
        sq = self.r_kt[:, which % 2, :]
        P.act(sq, raw, AF.Square)
        ms = P.psb()
        P.mm(ms, self.blk64b, sq)
        self._rs(d_, ms)
        P.stt(dst, raw, wcol, d_, ALU.mult, ALU.mult)
        yield
    n_att = 0
    for j in range(2):
        o_all = P.psl(2)
        den = P.psl(3)
        for i in range(NB):
            bs = slice(i * 128, (i + 1) * 128)
            first = (g == 0 and i == 0)
            for hh in range(2):
                h = 2 * j + hh
                kv = j
                rs_ = slice(kv * 64, (kv + 1) * 64)
                ST = P.psb()
                for s_ in range(2):
                    kcols = slice(i * 128 + s_ * 128, i * 128 + s_ * 128 + 128)
                    P.mm(ST[:, s_ * 128:(s_ + 1) * 128], khat[rs_, kcols], qh[rs_, h % 2, bs], start=True, stop=False)
                    bb = self.biasb[:, 8, :] if (first and s_ == 0) else self.biasb[:, h * 2 + s_, :]
                    P.mm(ST[:, s_ * 128:(s_ + 1) * 128], self.identb, bb, start=False, stop=True)
                PT = self.r_att[:, n_att % 2, :]
                n_att += 1
                P.act(PT, ST[:, 0:256], AF.Exp, scale=0.125)
                yield
                for s_ in range(2):
                    vv = vp[:, i + s_, :].rearrange("p (v n) -> p v n", v=4)[:, 2 * kv + hh, :]
                    st_ = (hh == 0 and s_ == 0)
                    sp_ = (hh == 1 and s_ == 1)
                    P.mm(o_all[:, bs], vv, PT[:, s_ * 128:(s_ + 1) * 128], start=st_, stop=sp_)
                    P.mm(den[:, bs], self.onespad[:, hh, :], PT[:, s_ * 128:(s_ + 1) * 128], start=st_, stop=sp_)
            self._gates_some(l, 1)
            yield
        P.ts(a_, den, dc[:, 4 + j:5 + j], None, ALU.add)
        P.op("dve", lambda e, a_=a_, b_=b_: e.reciprocal(b_, a_), [b_], [a_])
        P.tt(yT[:, j, :], o_all, b_, ALU.mult)
        yield
    P.copy(khat[:, 0:128], khat[:, G:G + 128], eng="pool")
    P.copy(self.vcar[:, l, :], vp[:, NB, :], eng="pool")
    yield


def _rglru(self, l, g, j, temps, lgraw, xcb):
    P = self.P
    dc = self.dcols[:, l, :]
    yT = self.yT[3]
    a_, b_, c_, d_, e_ = temps
    wt = self._fm_load(l, 15 + j, 1)
    P.copy(self.lxh[:, j, 0:3], self.lxc[:, l * 2 + j, 0:3], eng="pool")
    P.copy(self.lxh[:, j, 3:3 + G], self._proj(wt, 0))
    wt = self._fm_load(l, 17 + j, 1)
    P.copy(lgraw, self._proj(wt, 0))
    yield
    lx = self.lxh[:, j, :]
    cw = self.pc(("conv_w", l), j * 4, 4)
    xc = a_
    P.ts(xc, lx[:, 0:G], cw[:, 0:1], self.pc(("conv_b", l), j), ALU.mult, ALU.add)
    for jt in range(1, 4):
        P.stt(xc, lx[:, jt:jt + G], cw[:, jt:jt + 1], xc, ALU.mult, ALU.add)
    P.copy(xcb, xc, eng="pool")
    yield
    pr = P.psb()
    pi_ = P.psb()
    P.mm(pr, self.lruw[:, l * 4 + j, :], xcb)
    P.mm(pi_, self.lruw[:, l * 4 + 2 + j, :], xcb)
    P.act(b_, pr, AF.Sigmoid, bias=self.pc(("lru_ba", l), j))
    P.act(c_, pi_, AF.Sigmoid, bias=self.pc(("lru_bx", l), j))
    xg = lgraw
    P.act(d_, xg, AF.Square)
    P.ts(d_, d_, 0.044715, 1.0, ALU.mult, ALU.add)
    P.tt(d_, d_, xg, ALU.mult)
    P.act(d_, d_, AF.Tanh, scale=0.7978845608028654)
    yield
    P.stt(xg, d_, 1.0, xg, ALU.add, ALU.mult)
    P.tt(c_, c_, xc, ALU.mult, eng="pool")
    P.act(d_, b_, AF.Exp, scale=dc[:, 6 + j:7 + j])
    P.act(e_, b_, AF.Exp, scale=dc[:, 8 + j:9 + j])
    P.act(e_, e_, AF.Ln, scale=-1.0, bias=1.0)
    P.act(e_, e_, AF.Exp, scale=0.5)
    yield
    P.tt(c_, c_, e_, ALU.mult, eng="pool")
    hcol = self.hst[:, l, j:j + 1]
    P.scan(b_, d_, c_, hcol, ALU.mult, ALU.add)
    P.copy(hcol, b_[:, G - 1:G], eng="pool")
    yield
    P.stt(yT[:, j, :], b_, 0.5, lgraw, ALU.mult, ALU.mult)
    P.copy(self.lxc[:, l * 2 + j, 0:3], lx[:, G:G + 3], eng="pool")
    self._gates_some(l, 2)
    yield


def _merge(self, l, g):
    P = self.P
    wbv = self.wbf[("wbr", l)].rearrange("(a p) n -> p a n", p=128)
    wov = self.wbf[("wout", l)].rearrange("(c p) n -> p c n", p=128)
    wts = []
    for hf in range(2):
        wt = self.wslot().rearrange("p (a n) -> p a n", a=4)
        self.wload(wt, wbv[:, hf * 4:(hf + 1) * 4, :])
        wts.append(wt)
    wo = self.wslot().rearrange("p (c n) -> p c n", c=NCH)
    self.wload(wo, wov[:, :, 0:512])
    pss = [P.psl(k_) for k_ in range(4)]
    accs = [[self.m_a, self.m_b, self.m_c, self.m_d], [self.r_a, self.r_b, self.r_c, self.r_d],
            [self.m_q[:, 0, :], self.m_q[:, 1, :], self.m_z[:, 0, :], self.m_z[:, 1, :]]]

    def stage1(cch):
        acc = accs[cch % 3]
        for n in range(4):
            gt = self.gatesb[:, n * 8 + cch, :]
            P.act(gt, gt, AF.Sigmoid, bias=self.pc(("gate_bias", l), n * 8 + cch))
        for n in range(4):
            wt = wts[n // 2]
            ps = P.psb()
            for kc in range(2):
                P.mm(ps, wt[:, (n % 2) * 2 + kc, cch * 128:(cch + 1) * 128], self.yT[n][:, kc, :], start=(kc == 0), stop=(kc == 1))
            P.tt(acc[n], ps, self.gatesb[:, n * 8 + cch, :], ALU.mult)

    def stage2(cch):
        acc = accs[cch % 3]
        P.tt(acc[0], acc[0], acc[1], ALU.add, eng="pool")
        P.tt(acc[2], acc[2], acc[3], ALU.add, eng="pool")
        P.tt(self.merged[:, cch, :], acc[0], acc[2], ALU.add, eng="pool")

    stage1(0)
    stage1(1)
    for cch in range(NCH):
        stage2(cch)
        if cch + 2 < NCH:
            stage1(cch + 2)
        for m in range(4):
            P.mm(pss[m], wo[:, cch, m * 128:(m + 1) * 128], self.merged[:, cch, :], start=(cch == 0), stop=(cch == NCH - 1))
    for m in range(4):
        P.tt(self.xT[:, m, :], pss[m], self.xT[:, m, :], ALU.add)
    wo = self.wslot().rearrange("p (c n) -> p c n", c=NCH)
    self.wload(wo, wov[:, :, 512:1024])
    for m in range(4):
        ps = P.psb()
        for cc in range(NCH):
            P.mm(ps, wo[:, cc, m * 128:(m + 1) * 128], self.merged[:, cc, :], start=(cc == 0), stop=(cc == NCH - 1))
        P.tt(self.xT[:, 4 + m, :], ps, self.xT[:, 4 + m, :], ALU.add)


Builder.mixer_setup = _mixer_setup
Builder._gates_some = _gates_some
Builder._fm_load = _fm_load
Builder._proj = _proj
Builder.mixer = _mixer
Builder._rs = _rs
Builder.hgrn = _hgrn
Builder.retention = _retention
Builder.swa = _swa
Builder.rglru = _rglru
Builder.merge = _merge


def host_consts(inp):
    ct, cb = const_table()
    lw = np.zeros((NL, 4, 128, 128), np.float32)
    for l in range(NL):
        for j in range(2):
            for k, nm in ((0, "lru_wa"), (2, "lru_wx")):
                w = np.asarray(inp[nm][l])
                lw[l, k + j, 0:64, 0:64] = w[2 * j]
                lw[l, k + j, 64:128, 64:128] = w[2 * j + 1]
    return {"ctab": ct, "cbf": cb, "lruw": lw}


def kernel(**inputs):
    x = np.asarray(inputs["x"])
    Bn, T, _ = x.shape
    b = Builder(T)
    nc = b.build()
    consts = host_consts(inputs)
    in_maps = []
    for i in range(Bn):
        m = host_inputs(inputs, i)
        m.update(consts)
        in_maps.append(m)
    res = run_bass_kernel_spmd(nc, in_maps, core_ids=list(range(Bn)))
    out = np.stack([np.asarray(r["outT"]).T for r in res.results], axis=0)
    return np.ascontiguousarray(out.astype(np.float32))
```

```python
import numpy as np
import concourse.bass as bass
import concourse.mybir as mybir
from concourse.bass_utils import run_bass_kernel_spmd

F32 = mybir.dt.float32
BF16 = mybir.dt.bfloat16
AF = mybir.ActivationFunctionType
ALU = mybir.AluOpType

D = 1024
DFF = 2816
NL = 2
G = 512
NCH = 8
NHC = 22
EPS = 1e-6
ENGS = ("pe", "act", "dve", "pool", "sp")


class _Op:
    __slots__ = ("eng", "fn", "deps", "signal", "sigval", "dma_key", "dma_val")

    def __init__(self, eng, fn):
        self.eng = eng
        self.fn = fn
        self.deps = []
        self.signal = False
        self.sigval = 0
        self.dma_key = None
        self.dma_val = 0


class Sched:
    def __init__(self):
        self.ops = {e: [] for e in ENGS}
        self.last_w = {}
        self.readers = {}
        self.dma_cnt = {}
        self.batch_keys = set()

    def add(self, eng, fn, reads=(), writes=(), dma_key=None, batch=False):
        op = _Op(eng, fn)
        deps = []
        for r in reads:
            t = self.last_w.get(r)
            if t is not None:
                deps.append(t)
        for w in writes:
            t = self.last_w.get(w)
            if t is not None:
                deps.append(t)
            deps.extend(self.readers.get(w, ()))
        seen = set()
        for d in deps:
            if id(d) in seen or d is op:
                continue
            seen.add(id(d))
            if d.dma_key is None and d.eng == eng and eng == "pe":
                continue
            op.deps.append(d)
            if d.dma_key is None:
                d.signal = True
        if dma_key is not None:
            op.dma_key = dma_key
            self.dma_cnt[dma_key] = self.dma_cnt.get(dma_key, 0) + 16
            op.dma_val = self.dma_cnt[dma_key]
            if batch:
                self.batch_keys.add(dma_key)
        for r in reads:
            self.readers.setdefault(r, []).append(op)
        for w in writes:
            self.last_w[w] = op
            self.readers[w] = []
        self.ops[eng].append(op)
        return op

    def emit(self, nc, final_waits=()):
        for e in ENGS:
            n = 0
            for op in self.ops[e]:
                if op.dma_key is None and op.signal:
                    n += 1
                    op.sigval = n
        import contextlib
        with contextlib.ExitStack() as st:
            esem = {e: st.enter_context(nc.semaphore("s_" + e)) for e in ENGS}
            dsem = {k: st.enter_context(nc.semaphore("d_%s" % (str(k).replace(" ", "").replace("'", "").replace(",", "_").replace("(", "").replace(")", ""))))
                    for k in self.dma_cnt}
            block = st.enter_context(nc.Block())
            sched = self

            def run(e):
                def body(engine):
                    waited = {}
                    for op in sched.ops[e]:
                        for d in op.deps:
                            if d.dma_key is not None:
                                sem = dsem[d.dma_key]
                                val = sched.dma_cnt[d.dma_key] if d.dma_key in sched.batch_keys else d.dma_val
                                key = ("d", d.dma_key)
                            else:
                                sem = esem[d.eng]
                                val = d.sigval
                                key = ("e", d.eng)
                            if waited.get(key, 0) >= val:
                                continue
                            waited[key] = val
                            engine.wait_ge(sem, val)
                        ins = op.fn(engine)
                        if op.dma_key is not None:
                            ins.then_inc(dsem[op.dma_key], 16)
                        elif op.signal:
                            ins.then_inc(esem[e], 1)
                    if e == "sp":
                        for k in final_waits:
                            engine.wait_ge(dsem[k], sched.dma_cnt[k])
                return body

            block.tensor(run("pe"))
            block.scalar(run("act"))
            block.vector(run("dve"))
            block.gpsimd(run("pool"))
            block.sync(run("sp"))


CELL = 256


def _esz(dt):
    return 4 if dt == F32 else 2


def _cells(ap):
    name = ap.tensor.name
    if name in ("xT", "outT"):
        return [(name, ap.offset)]
    if name not in ("SB", "PS"):
        return [name]
    es = _esz(ap.dtype)
    pat = ap.ap
    pstep = pat[0][0]
    first = ap.offset % pstep if pstep else ap.offset
    last = first
    for st, cnt in pat[1:]:
        last += st * (cnt - 1)
    return [(name, c) for c in range(first * es // CELL, (last * es + es - 1) // CELL + 1)]


class Prog:
    def __init__(self, nc, sb_bytes):
        self.nc = nc
        self.S = Sched()
        self.SB = nc.alloc_sbuf_tensor("SB", [128, sb_bytes // 4], F32).ap()
        self.PS = nc.alloc_psum_tensor("PS", [128, 4096], F32).ap()
        self.sb_bytes = sb_bytes
        self.off = 0
        self.bank = 0

    def alloc(self, nbytes, dt, shape=None):
        nbytes = (nbytes + CELL - 1) // CELL * CELL
        o = self.off
        self.off += nbytes
        assert self.off <= self.sb_bytes, ("SBUF overflow", self.off)
        v = self.SB[:, o // 4:(o + nbytes) // 4]
        if dt != F32:
            v = v.bitcast(dt)
        return v

    def tile(self, dt, *shape):
        n = 1
        for s_ in shape:
            n *= s_
        v = self.alloc(n * _esz(dt), dt)[:, 0:n]
        if len(shape) == 2:
            return v.rearrange("p (a b) -> p a b", a=shape[0])
        if len(shape) == 3:
            return v.rearrange("p (a b c) -> p a b c", a=shape[0], b=shape[1])
        if len(shape) == 4:
            return v.rearrange("p (a b c d) -> p a b c d", a=shape[0], b=shape[1], c=shape[2])
        return v

    nrot = 4

    def psb(self, n=1):
        if self.bank + n > self.nrot:
            self.bank = 0
        b = self.bank
        self.bank = (self.bank + n) % self.nrot
        return self.PS[:, b * 512:(b + n) * 512]

    def psl(self, k):
        return self.PS[:, (4 + k) * 512:(5 + k) * 512]

    def op(self, eng, fn, outs, ins, dma_key=None, batch=False):
        r = []
        for a in ins:
            r.extend(_cells(a))
        w = []
        for a in outs:
            w.extend(_cells(a))
        return self.S.add(eng, fn, reads=r, writes=w, dma_key=dma_key, batch=batch)

    def mm(self, out, lhsT, rhs, start=True, stop=True):
        self.op("pe", lambda e: e.matmul(out, lhsT, rhs, start=start, stop=stop), [out], [lhsT, rhs] + ([] if start else [out]))

    def tr(self, out, in_, ident):
        self.op("pe", lambda e: e.transpose(out, in_, ident), [out], [in_, ident])

    def act(self, out, in_, func, bias=None, scale=None, eng="act"):
        ins = [in_]
        kw = {}
        if bias is not None:
            kw["bias"] = bias
            if not isinstance(bias, (int, float)):
                ins.append(bias)
        if scale is not None:
            kw["scale"] = scale
            if not isinstance(scale, (int, float)):
                ins.append(scale)
        self.op(eng, lambda e: e.activation(out, in_, func, **kw), [out], ins)

    def tt(self, out, a, b, op, eng="dve"):
        self.op(eng, lambda e: e.tensor_tensor(out, a, b, op), [out], [a, b])

    def ts(self, out, a, s1, s2, op0, op1=None, eng="dve"):
        ins = [a] + [s for s in (s1, s2) if s is not None and not isinstance(s, (int, float))]
        if op1 is None:
            self.op(eng, lambda e: e.tensor_scalar(out, a, s1, None, op0), [out], ins)
        else:
            self.op(eng, lambda e: e.tensor_scalar(out, a, s1, s2, op0, op1), [out], ins)

    def stt(self, out, a, sc, b, op0, op1, eng="dve"):
        ins = [a, b] + ([] if isinstance(sc, (int, float)) else [sc])
        self.op(eng, lambda e: e.scalar_tensor_tensor(out, a, sc, b, op0, op1), [out], ins)

    def copy(self, out, a, eng="dve"):
        self.op(eng, lambda e: e.tensor_copy(out, a), [out], [a])

    def memset(self, out, val, eng="pool"):
        self.op(eng, lambda e: e.memset(out, val), [out], [])

    def scan(self, out, d0, d1, init, op0, op1, eng="dve"):
        ins = [d0, d1] + ([] if isinstance(init, (int, float)) else [init])
        self.op(eng, lambda e: e.tensor_tensor_scan(out, d0, d1, init, op0, op1), [out], ins)

    def dma(self, eng, out, in_, key, batch=False):
        self.op(eng, lambda e: e.dma_start(out=out, in_=in_), [out], [in_], dma_key=key, batch=batch)


def _pc_layout():
    names = {}
    n = 0

    def add(key, cnt):
        nonlocal n
        names[key] = n
        n += cnt
    for l in range(NL):
        add(("ffn1_norm", l), 8)
        add(("mix_norm", l), 8)
        add(("ffn2_norm", l), 8)
        add(("gate_bias", l), 32)
        add(("lb_logit", l), 2)
        add(("hg_outw", l), 2)
        add(("gn_w", l), 2)
        add(("gn_b", l), 2)
        add(("qn_w", l), 1)
        add(("kn_w", l), 1)
        add(("sink", l), 2)
        add(("conv_w", l), 8)
        add(("conv_b", l), 2)
        add(("lru_ba", l), 2)
        add(("lru_bx", l), 2)
        add(("lru_lam", l), 2)
    return names, n


PC, NPC = _pc_layout()


def _fm(v):
    return np.ascontiguousarray(np.asarray(v, np.float32).reshape(-1, 128).T)


class Builder:
    def __init__(self, T, phases=("ffn1", "mix", "ffn2"), n_layers=NL, dbg=None):
        self.T = T
        self.NG = T // G
        self.phases = phases
        self.n_layers = n_layers
        self.dbg = dbg
        nc = self.nc = bass.Bass("TRN2", target_bir_lowering=False)
        self.P = P = Prog(nc, 207 * 1024)
        dt = nc.dram_tensor
        self.x_in = dt("xT", [D, T], F32, kind="ExternalInput").ap()
        self.out = dt("outT", [D, T], F32, kind="ExternalOutput").ap()
        self.pcols_d = dt("pcols", [128, NPC], F32, kind="ExternalInput").ap()
        self.w32 = {}
        self.wbf = {}
        for nm, shp in (("wg1", [D, DFF]), ("wu1", [D, DFF]), ("wd1", [DFF, D]),
                        ("win", [D, WIN_COLS]), ("wbr", [1024, D]), ("wout", [D, D]),
                        ("wg2", [D, DFF]), ("wu2", [D, DFF]), ("wd2", [DFF, D])):
            for l in range(n_layers):
                self.w32[(nm, l)] = dt("%s_%d" % (nm, l), shp, F32, kind="ExternalInput").ap()
                self.wbf[(nm, l)] = dt("b_%s_%d" % (nm, l), shp, BF16, kind="Internal").ap()
        self.xT = P.tile(F32, NCH, G)
        self.hT = P.tile(BF16, NCH, G)
        self.pcols = P.tile(F32, 1, NPC)[:, 0, :]
        self.big = P.alloc(32768, BF16)
        self.sqb = self.big[:, 0:NCH * G].rearrange("p (c n) -> p c n", c=NCH)
        self.wslots = [P.alloc(8192, BF16) for _ in range(3)]
        self.wslot_i = 0
        self.m_a, self.m_b, self.m_c, self.m_d, self.m_e = [P.tile(F32, 1, G)[:, 0, :] for _ in range(5)]
        self.rstd = self.m_e
        self.onesD = P.tile(BF16, 1, 128)[:, 0, :]
        self.cst32 = P.tile(F32, 1, 128)[:, 0, :]

    def pc(self, key, j=0, n=1):
        o = PC[key] + j
        return self.pcols[:, o:o + n]

    def wslot(self):
        s = self.wslots[self.wslot_i]
        self.wslot_i = (self.wslot_i + 1) % len(self.wslots)
        return s

    def wload(self, dst, src):
        self.P.dma("sp", dst, src, key=("w", id(dst.tensor), dst.offset))

    def convert_all(self):
        P = self.P
        order = ("wg1", "wu1", "wd1", "win", "wbr", "wout", "wg2", "wu2", "wd2")
        for l in range(self.n_layers):
            for nm in order:
                P.dma("pool", self.wbf[(nm, l)], self.w32[(nm, l)], key=("cv", nm, l))

    def consts(self):
        P = self.P
        P.dma("act", self.pcols, self.pcols_d, key="c0")
        P.memset(self.cst32, 1.0 / D)
        P.copy(self.onesD, self.cst32, eng="pool")

    def rmsnorm(self, wkey, l):
        P = self.P
        ps = P.psb()
        for c in range(NCH):
            P.act(self.sqb[:, c, :], self.xT[:, c, :], AF.Square)
        for c in range(NCH):
            P.mm(ps, self.onesD, self.sqb[:, c, :], start=(c == 0), stop=(c == NCH - 1))
        P.act(self.rstd, ps, AF.Ln, bias=self.eps_col)
        P.act(self.rstd, self.rstd, AF.Exp, scale=-0.5)
        for c in range(NCH):
            P.stt(self.hT[:, c, :], self.xT[:, c, :], self.pc((wkey, l), c), self.rstd, ALU.mult, ALU.mult)

    def ffn(self, l, which):
        P = self.P
        wg, wu, wd = self.wbf[("wg" + which, l)], self.wbf[("wu" + which, l)], self.wbf[("wd" + which, l)]
        self.rmsnorm("ffn%s_norm" % which, l)
        actT = self.big[:, 0:NHC * G].rearrange("p (j n) -> p j n", j=NHC)
        wgv = wg.rearrange("(c p) n -> p c n", p=128)
        wuv = wu.rearrange("(c p) n -> p c n", p=128)
        P.nrot = 8
        P.bank = 0
        for jp in range(NHC // 2):
            slot = self.wslot()
            wt = slot.rearrange("p (u c n) -> p u c n", u=2, c=NCH)
            self.wload(wt[:, 0], wgv[:, :, jp * 256:(jp + 1) * 256])
            self.wload(wt[:, 1], wuv[:, :, jp * 256:(jp + 1) * 256])
            for jj in range(2):
                j = jp * 2 + jj
                pg = P.psb()
                pu = P.psb()
                for c in range(NCH):
                    P.mm(pg, wt[:, 0, c, jj * 128:(jj + 1) * 128], self.hT[:, c, :], start=(c == 0), stop=(c == NCH - 1))
                for c in range(NCH):
                    P.mm(pu, wt[:, 1, c, jj * 128:(jj + 1) * 128], self.hT[:, c, :], start=(c == 0), stop=(c == NCH - 1))
                sg = self.tmps[j % 3]
                P.act(sg, pg, AF.Silu)
                P.tt(actT[:, j, :], pu, sg, ALU.mult)
        P.nrot = 4
        P.bank = 0
        wdv = wd.rearrange("(j p) n -> p j n", p=128)
        for half in range(2):
            pss = [P.psl(k_) for k_ in range(4)]
            j0 = 0
            for nj in (8, 8, 6):
                slot = self.wslot()
                wt = slot.rearrange("p (j n) -> p j n", j=8)
                self.wload(wt[:, 0:nj], wdv[:, j0:j0 + nj, half * 512:(half + 1) * 512])
                for jj in range(nj):
                    j = j0 + jj
                    for m in range(4):
                        P.mm(pss[m], wt[:, jj, m * 128:(m + 1) * 128], actT[:, j, :], start=(j == 0), stop=(j == NHC - 1))
                j0 += nj
            for m in range(4):
                c = half * 4 + m
                P.stt(self.xT[:, c, :], pss[m], 0.5, self.xT[:, c, :], ALU.mult, ALU.add)

    def build(self):
        P = self.P
        self.eps_col = P.tile(F32, 1, 1)[:, 0, :]
        self.tmps = [self.m_a, self.m_b, self.m_c]
        P.memset(self.eps_col, EPS)
        self.consts()
        self.convert_all()
        if "mix" in self.phases:
            self.mixer_setup()
        xv = self.x_in.rearrange("(c p) t -> p c t", p=128)
        ov = self.out.rearrange("(c p) t -> p c t", p=128)
        for g in range(self.NG):
            for cx in range(NCH):
                P.dma("pool", self.xT[:, cx, :], xv[:, cx, g * G:(g + 1) * G], key=("xin", cx))
            for l in range(self.n_layers):
                if "ffn1" in self.phases:
                    self.ffn(l, "1")
                if "mix" in self.phases:
                    self.mixer(l, g)
                if "ffn2" in self.phases:
                    self.ffn(l, "2")
            for cx in range(NCH):
                P.dma("pool", ov[:, cx, g * G:(g + 1) * G], self.xT[:, cx, :], key=("xout", cx))
        P.S.emit(self.nc, final_waits=[("xout", cx) for cx in range(NCH)])
        return self.nc


WIN_COLS = 51 * 128 + 896


def _win_perm():
    o = {}
    names = ("hq", "hf", "hi", "hg", "rq", "rk", "rv", "rg", "aq", "ak", "av", "lx", "lg", "gate")
    sizes = (256, 256, 256, 256, 256, 256, 256, 256, 256, 128, 128, 256, 256, 4096)
    s = 0
    for n_, z in zip(names, sizes):
        o[n_] = s
        s += z
    r = lambda n_, a=0, b=None: list(range(o[n_] + a, o[n_] + (b if b is not None else dict(zip(names, sizes))[n_])))
    cols = []
    cols += r("gate")
    cols += r("hq") + r("hf") + r("hg")
    cols += r("rq") + r("rk") + r("rg")
    cols += r("aq", 0, 64) + r("aq", 128, 192) + r("aq", 64, 128) + r("aq", 192, 256)
    cols += r("ak")
    cols += r("lx") + r("lg")
    cols += r("rk") + r("rv") + r("hi") + r("av")
    return np.array(cols, np.int64)


def host_inputs(inp, b, T=None):
    x = np.asarray(inp["x"])[b]
    if T is not None:
        x = x[:T]
    m = {"xT": np.ascontiguousarray(x.T)}
    pc = np.zeros((128, NPC), np.float32)
    perm = _win_perm()
    for l in range(NL):
        def put(key, arr):
            a = _fm(arr)
            pc[:, PC[(key, l)]:PC[(key, l)] + a.shape[1]] = a
        put("ffn1_norm", inp["ffn1_norm"][l])
        put("mix_norm", inp["mix_norm"][l])
        put("ffn2_norm", inp["ffn2_norm"][l])
        put("gate_bias", np.asarray(inp["gate_bias"][l]).reshape(-1))
        put("lb_logit", inp["hgrn_lb_logits"][l])
        put("hg_outw", inp["hgrn_out_norm"][l])
        put("gn_w", inp["ret_gn_w"][l])
        put("gn_b", inp["ret_gn_b"][l])
        put("qn_w", np.tile(np.asarray(inp["attn_q_norm"][l]), 2))
        put("kn_w", np.tile(np.asarray(inp["attn_k_norm"][l]), 2))
        put("sink", np.repeat(np.asarray(inp["attn_sinks"][l]), 64))
        cw = np.asarray(inp["lru_conv_w"][l])
        put("conv_w", np.concatenate([cw[:, 0:128].reshape(-1), cw[:, 128:256].reshape(-1)]).reshape(8, 128).reshape(-1))
        put("conv_b", inp["lru_conv_b"][l])
        put("lru_ba", inp["lru_ba"][l])
        put("lru_bx", inp["lru_bx"][l])
        put("lru_lam", inp["lru_lambda"][l])
        m["wg1_%d" % l] = np.asarray(inp["ffn1_wg"][l])
        m["wu1_%d" % l] = np.asarray(inp["ffn1_wu"][l])
        m["wd1_%d" % l] = np.asarray(inp["ffn1_wd"][l])
        m["wg2_%d" % l] = np.asarray(inp["ffn2_wg"][l])
        m["wu2_%d" % l] = np.asarray(inp["ffn2_wu"][l])
        m["wd2_%d" % l] = np.asarray(inp["ffn2_wd"][l])
        m["win_%d" % l] = np.ascontiguousarray(np.asarray(inp["w_in"][l])[:, perm])
        m["wbr_%d" % l] = np.ascontiguousarray(np.asarray(inp["w_branch"][l]).reshape(1024, D))
        m["wout_%d" % l] = np.asarray(inp["w_out"][l])
    m["pcols"] = pc
    return m


def _ct_layout():
    names = {}
    n = 0

    def add(key, cnt):
        nonlocal n
        names[key] = (n, cnt)
        n += cnt
    add("ident", 128)
    add("blk64", 128)
    add("bdmask", 128)
    add("reset", G)
    add("cmask8", 8)
    add("hmaskT2", 256)
    for p in range(2):
        add(("decayT", p), 256)
        add(("qdecay", p), 128)
    add("kdecay", 256)
    add("g128", 2)
    return names, n


CT, NCT = _ct_layout()
NCB = 128 + 128 + 9 * 128 + 256


def const_table():
    t = np.zeros((128, NCT), np.float64)

    def put(key, arr):
        o, c = CT[key]
        t[:, o:o + c] = arr
    idx = np.arange(128)
    put("ident", np.eye(128))
    blk = (idx[:, None] // 64 == idx[None, :] // 64).astype(np.float64)
    put("blk64", blk / 64.0)
    put("bdmask", blk)
    tt = np.arange(G)
    put("reset", np.broadcast_to((tt % 16 != 0).astype(np.float64), (128, G)))
    put("cmask8", (idx[:, None] // 16 == np.arange(8)[None, :]).astype(np.float64))
    hm = ((idx[:, None] // 16 == idx[None, :] // 16) & (idx[:, None] <= idx[None, :])).astype(np.float64)
    put("hmaskT2", np.concatenate([hm, hm], 1))
    lg = np.log1p(-np.exp2(-5.0 - np.arange(4)))
    rel = idx[None, :] - idx[:, None]
    kd = np.zeros((128, 256))
    g128 = np.zeros((128, 2))
    for p in range(2):
        dT = []
        qd = np.zeros((128, 128))
        for hh in range(2):
            h = 2 * p + hh
            dT.append(np.where(rel >= 0, np.exp(lg[h] * np.maximum(rel, 0)), 0.0) / 8.0)
            qd[hh * 64:(hh + 1) * 64, :] = np.exp(lg[h] * (idx[None, :] + 1.0)) / 8.0
            kd[:, h * 64:(h + 1) * 64] = np.exp(lg[h] * (127.0 - idx))[:, None]
            g128[hh * 64:(hh + 1) * 64, p] = np.exp(lg[h] * 128.0)
        put(("decayT", p), np.concatenate(dT, 1))
        put(("qdecay", p), qd)
    put("kdecay", kd)
    put("g128", g128)
    tb = np.zeros((128, NCB), np.float64)
    tb[:, 0:128] = np.eye(128)
    tb[:, 128:256] = blk / 64.0
    slopes = np.exp2(-8.0 * np.arange(1, 5) / 4.0)
    NEG = -240000.0
    kk = idx[:, None]
    qq = idx[None, :]
    for h in range(4):
        prev = np.where(kk > qq, -slopes[h] * (qq + 128 - kk) * 8.0, NEG)
        cur = np.where(kk <= qq, -slopes[h] * (qq - kk) * 8.0, NEG)
        tb[:, 256 + (2 * h) * 128:256 + (2 * h + 1) * 128] = prev
        tb[:, 256 + (2 * h + 1) * 128:256 + (2 * h + 2) * 128] = cur
    tb[:, 256 + 8 * 128:256 + 9 * 128] = NEG
    op = np.zeros((128, 256))
    op[:, 0:64] = 1.0
    op[:, 128 + 64:256] = 1.0
    tb[:, 256 + 9 * 128:] = op
    return t.astype(np.float32), tb.astype(np.float32)


NB = G // 128
import os
SKIP = os.environ.get('SKIP', '')
STOP = int(os.environ.get('STOP', '99'))
FMC = 19


def _mixer_setup(self):
    P = self.P
    nc = self.nc
    self.ctab_d = nc.dram_tensor("ctab", [128, NCT], F32, kind="ExternalInput").ap()
    self.lruw_d = nc.dram_tensor("lruw", [NL, 4, 128, 128], F32, kind="ExternalInput").ap()
    self.ct = P.tile(F32, 1, NCT)[:, 0, :]
    P.dma("act", self.ct, self.ctab_d, key="c3")
    c = lambda key: self.ct[:, CT[key][0]:CT[key][0] + CT[key][1]]
    self.c = c
    self.cbf_d = nc.dram_tensor("cbf", [128, NCB], F32, kind="ExternalInput").ap()
    cb = P.tile(BF16, 1, NCB)[:, 0, :]
    P.dma("pool", cb, self.cbf_d, key="c1")
    self.identb = cb[:, 0:128]
    self.blk64b = cb[:, 128:256]
    self.biasb = cb[:, 256:256 + 9 * 128].rearrange("p (a n) -> p a n", a=9)
    self.onespad = cb[:, 256 + 9 * 128:].rearrange("p (a n) -> p a n", a=2)
    self.lruw = P.tile(BF16, NL * 4, 128)
    P.dma("pool", self.lruw, self.lruw_d.rearrange("l j p n -> p (l j) n"), key="c2")
    self.dcols = P.tile(F32, NL, 16)
    for l in range(self.n_layers):
        dc = self.dcols[:, l, :]
        if l == 0:
            P.memset(dc[:, 0:2], 0.0)
        else:
            P.tt(dc[:, 10:12], self.pc(("lb_logit", 1), 0, 2), self.pc(("lb_logit", 0), 0, 2), ALU.subtract)
            P.act(dc[:, 0:2], dc[:, 10:12], AF.Sigmoid)
        P.ts(dc[:, 2:4], dc[:, 0:2], -1.0, 1.0, ALU.mult, ALU.add)
        P.act(dc[:, 4:6], self.pc(("sink", l), 0, 2), AF.Exp)
        P.act(dc[:, 12:14], self.pc(("lru_lam", l), 0, 2), AF.Exp, scale=-1.0)
        P.act(dc[:, 12:14], dc[:, 12:14], AF.Ln, bias=1.0)
        P.ts(dc[:, 6:8], dc[:, 12:14], -8.0, None, ALU.mult)
        P.ts(dc[:, 8:10], dc[:, 12:14], -16.0, None, ALU.mult)
    L = self.n_layers
    self.Sh = P.tile(F32, 9, 128)
    self.Sc = P.tile(F32, L * 2, 128)
    self.Sr = P.tile(F32, L * 2, 128)
    self.khat = P.tile(BF16, L, 128 + G)
    self.vpad = P.tile(BF16, NB + 1, 4 * 128)
    self.vcar = P.tile(BF16, L, 4 * 128)
    self.lxh = P.tile(F32, 2, G + 4)
    self.lxc = P.tile(F32, L * 2, 4)
    self.hst = P.tile(F32, L, 2)
    for t_ in (self.Sh, self.Sc, self.Sr, self.khat, self.vpad, self.vcar, self.lxh, self.lxc, self.hst):
        P.memset(t_, 0.0)
    self.gatesb = self.big[:, 0:32 * G].rearrange("p (j n) -> p j n", j=32)
    f32t = lambda *s: P.tile(F32, *s)
    bft = lambda *s: P.tile(BF16, *s)
    self.m_q = f32t(2, G)
    self.m_z = f32t(2, G)
    self.m_sg = f32t(2, G)
    self.m_qd = f32t(2, G)
    self.m_kpp = self.m_e
    self.m_qt = bft(2, G)
    self.m_kt = bft(2, G)
    self.m_dcol = f32t(2, 32)
    self.m_km8 = bft(8, 128)
    self.m_dSm = f32t(8, 128)
    self.m_att = bft(2, 256)
    self.m_tok = bft(NB, 256)
    self.m_vtok = bft(NB, 256)
    self.m_pad = bft(NB, 4 * 128)
    self.m_tmp128 = f32t(1, 128)[:, 0, :]
    self.m_lg = self.m_q
    self.hi_tok = bft(NB, 256)
    self.hi_pad = bft(NB, 4 * 128)
    self.gslots = [P.alloc(4096, BF16) for _ in range(2)]
    self.r_a, self.r_b, self.r_c, self.r_d = [f32t(1, G)[:, 0, :] for _ in range(4)]
    self.r_qd = f32t(2, G)
    self.r_qt = bft(2, G)
    self.r_kt = bft(2, G)
    self.r_sg = bft(2, G)
    self.r_att = bft(2, 256)
    P.memset(self.m_pad, 0.0)
    P.memset(self.hi_pad, 0.0)
    self.yT = [bft(2, G) for _ in range(4)]
    self.merged = self.hT
    self.acc = [self.m_a, self.m_b, self.m_c, self.m_d]


def _gates_some(self, l, k):
    P = self.P
    winv = self.wbf[("win", l)].rearrange("(c p) n -> p c n", p=128)
    while k > 0 and self.g_next < 32:
        j = self.g_next
        if j % 2 == 0:
            wt_ = self.gslots[(j // 2) % 2].rearrange("p (c n) -> p c n", c=NCH)
            self.wload(wt_, winv[:, :, j * 128:(j + 2) * 128])
            self.gate_slot = wt_
        wt = self.gate_slot
        ps = P.psb()
        cc = j % 2
        for c in range(NCH):
            P.mm(ps, wt[:, c, cc * 128:(cc + 1) * 128], self.hT[:, c, :], start=(c == 0), stop=(c == NCH - 1))
        P.copy(self.gatesb[:, j, :], ps)
        self.g_next += 1
        k -= 1


def _fm_load(self, l, m0, n):
    winv = self.wbf[("win", l)].rearrange("(c p) n -> p c n", p=128)
    wt = self.wslot().rearrange("p (c n) -> p c n", c=NCH)
    c0 = (32 + m0) * 128
    self.wload(wt[:, :, 0:n * 128], winv[:, :, c0:c0 + n * 128])
    return wt


def _proj(self, wt, k):
    P = self.P
    ps = P.psb()
    for cc in range(NCH):
        P.mm(ps, wt[:, cc, k * 128:(k + 1) * 128], self.hT[:, cc, :], start=(cc == 0), stop=(cc == NCH - 1))
    return ps


def _mixer(self, l, g):
    P = self.P
    c = self.c
    self.rmsnorm("mix_norm", l)
    winv = self.wbf[("win", l)].rearrange("(c p) n -> p c n", p=128)
    TM0 = 51 * 128
    slot0 = self.wslot().rearrange("p (c n) -> p c n", c=NCH)
    self.wload(slot0, winv[:, :, TM0:TM0 + 512])
    slot1 = self.wslot().rearrange("p (c n) -> p c n", c=NCH)
    self.wload(slot1[:, :, 0:384], winv[:, :, TM0 + 512:TM0 + 896])
    self.wload(slot1[:, :, 384:512], winv[:, :, TM0 + 768:TM0 + 896])
    vp = self.vpad
    P.copy(vp[:, 0, :], self.vcar[:, l, :], eng="pool")
    mpad = self.m_pad
    for i in range(NB):
        p0 = P.psb()
        p1 = P.psb()
        for cc in range(NCH):
            P.mm(p0, self.hT[:, cc, i * 128:(i + 1) * 128], slot0[:, cc, :], start=(cc == 0), stop=(cc == NCH - 1))
        for cc in range(NCH):
            P.mm(p1, self.hT[:, cc, i * 128:(i + 1) * 128], slot1[:, cc, :], start=(cc == 0), stop=(cc == NCH - 1))
        P.tt(self.m_tok[:, i, :], p0[:, 0:256], c("kdecay"), ALU.mult)
        P.copy(self.m_vtok[:, i, :], p0[:, 256:512])
        pad4 = lambda t_: bass.AP(t_.tensor, t_.offset, [list(t_.ap[0]), [256, 2], [192, 2], [1, 64]])
        P.copy(pad4(mpad[:, i, 0:64]), p0[:, 256:512].rearrange("p (a s n) -> p a s n", a=2, s=2))
        P.copy(self.hi_tok[:, i, :], p1[:, 0:256])
        P.copy(pad4(self.hi_pad[:, i, 0:64]), p1[:, 0:256].rearrange("p (a s n) -> p a s n", a=2, s=2))
        P.copy(pad4(vp[:, i + 1, 0:64]), p1[:, 256:384].rearrange("p (k n) -> p k n", k=2).unsqueeze(2).to_broadcast([128, 2, 2, 64]))
    self.g_next = 0

    def chain(*gens):
        for g_ in gens:
            yield from g_
    lru0 = self.rglru(l, g, 0, (self.m_a, self.m_b, self.m_c, self.m_d, self.m_e), self.m_q[:, 0, :], self.m_kt[:, 0, :])
    lru1 = self.rglru(l, g, 1, (self.r_a, self.r_b, self.r_c, self.r_d, self.r_qd[:, 0, :]), self.r_qd[:, 1, :], self.r_kt[:, 1, :])
    streams = [chain(self.hgrn(l, g), lru0), chain(self.retention(l, g), self.swa(l, g), lru1)]
    while streams:
        for s_ in list(streams):
            try:
                next(s_)
            except StopIteration:
                streams.remove(s_)
    self._gates_some(l, 32)
    self.merge(l, g)


def _rs(self, out, ps):
    P = self.P
    P.act(out, ps, AF.Ln, bias=self.eps_col)
    P.act(out, out, AF.Exp, scale=-0.5)


def _hgrn(self, l, g):
    P = self.P
    c = self.c
    dc = self.dcols[:, l, :]
    a_, b_, c_, d_, e_ = self.m_a, self.m_b, self.m_c, self.m_d, self.m_e
    yT = self.yT[0]
    wt = self._fm_load(l, 0, 4)
    for p in range(2):
        P.copy(self.m_q[:, p, :], self._proj(wt, p))
    for p in range(2):
        P.act(self.m_z[:, p, :], self._proj(wt, 2 + p), AF.Sigmoid)
    wt = self._fm_load(l, 4, 2)
    for p in range(2):
        P.act(self.m_sg[:, p, :], self._proj(wt, p), AF.Sigmoid)
    yield
    for p in range(2):
        q = self.m_q[:, p, :]
        f = self.m_z[:, p, :]
        Sh = self.Sh
        P.copy(Sh[:, 0, :], self.Sc[:, l * 2 + p, :], eng="pool")
        P.ts(f, f, dc[:, 2 + p:3 + p], dc[:, p:p + 1], ALU.mult, ALU.add)
        P.ts(a_, f, -1.0, 1.0, ALU.mult, ALU.add)
        P.act(e_, f, AF.Ln)
        P.scan(b_, c("reset"), e_, 0.0, ALU.mult, ALU.add)
        yield
        b3 = b_.rearrange("p (n j) -> p n j", j=16)
        P.tt(c_.rearrange("p (n j) -> p n j", j=16), b3, b3[:, :, 8:9].to_broadcast([128, G // 16, 16]), ALU.subtract)
        P.act(d_, c_, AF.Exp)
        P.tt(self.m_qt[:, p, :], q, d_, ALU.mult)
        yield
        P.act(d_, c_, AF.Exp, scale=-1.0)
        P.tt(self.m_kt[:, p, :], a_, d_, ALU.mult)
        P.act(d_, b_, AF.Exp)
        P.tt(self.m_qd[:, p, :], q, d_, ALU.mult)
        yield
        P.tt(c_.rearrange("p (n j) -> p n j", j=16), b3[:, :, 15:16].to_broadcast([128, G // 16, 16]), b3, ALU.subtract)
        P.act(d_, c_, AF.Exp)
        P.tt(self.m_kpp, a_, d_, ALU.mult)
        P.act(self.m_dcol[:, p, :], b3[:, :, 15], AF.Exp)
        yield
        o_all = P.psl(p)
        for i in range(NB):
            bs = slice(i * 128, (i + 1) * 128)
            kT = P.psb()
            P.tr(kT[:, 0:128], self.m_kpp[:, bs], c("ident"))
            P.tt(self.m_km8, kT[:, 0:128].unsqueeze(1).to_broadcast([128, 8, 128]),
                 c("cmask8").unsqueeze(2).to_broadcast([128, 8, 128]), ALU.mult)
            yield
            dS = P.psb(2)
            for cc in range(8):
                P.mm(dS[:, cc * 128:(cc + 1) * 128], self.m_km8[:, cc, :], self.hi_tok[:, i, p * 128:(p + 1) * 128])
            for hb in range(2):
                P.tt(self.m_dSm[:, hb * 4:(hb + 1) * 4, :], dS[:, hb * 512:(hb + 1) * 512].rearrange("p (a n) -> p a n", a=4),
                     c("bdmask").unsqueeze(1).to_broadcast([128, 4, 128]), ALU.mult)
            yield
            for cc in range(8):
                P.stt(Sh[:, cc + 1, :], Sh[:, cc, :], self.m_dcol[:, p, i * 8 + cc:i * 8 + cc + 1], self.m_dSm[:, cc, :], ALU.mult, ALU.add)
            aT = P.psb(2)
            for hh in range(2):
                rs_ = slice(hh * 64, (hh + 1) * 64)
                P.mm(aT[:, hh * 512:hh * 512 + 128], self.m_kt[rs_, p, bs], self.m_qt[rs_, p, bs])
            att = self.m_att[:, i % 2, :]
            P.tt(att.rearrange("p (a n) -> p a n", a=2), aT.rearrange("p (a n) -> p a n", a=2)[:, :, 0:128],
                 c("hmaskT2").rearrange("p (a n) -> p a n", a=2), ALU.mult)
            yield
            hp = self.hi_pad[:, i, :].rearrange("p (a s n) -> p a s n", a=2, s=2)
            P.mm(o_all[:, bs], hp[:, p, 0, :], att[:, 0:128], start=True, stop=False)
            P.mm(o_all[:, bs], hp[:, p, 1, :], att[:, 128:256], start=False, stop=False)
            for cc in range(8):
                cs = slice(i * 128 + cc * 16, i * 128 + cc * 16 + 16)
                P.mm(o_all[:, cs], Sh[:, cc, :], self.m_qd[:, p, cs], start=False, stop=(cc == 7))
            P.copy(Sh[:, 0, :], Sh[:, 8, :], eng="pool")
            self._gates_some(l, 1)
            yield
        P.copy(self.Sc[:, l * 2 + p, :], Sh[:, 0, :], eng="pool")
        sq = self.m_kt[:, p, :]
        P.act(sq, o_all, AF.Square)
        ms = P.psb()
        P.mm(ms, self.blk64b, sq)
        self._rs(d_, ms)
        yield
        P.stt(c_, o_all, self.pc(("hg_outw", l), p), d_, ALU.mult, ALU.mult)
        P.tt(yT[:, p, :], c_, self.m_sg[:, p, :], ALU.mult)
        yield


def _retention(self, l, g):
    P = self.P
    c = self.c
    yT = self.yT[1]
    a_, b_, c_, d_ = self.r_a, self.r_b, self.r_c, self.r_d
    qt, kt, qd, sg = self.r_qt, self.r_kt, self.r_qd, self.r_sg
    wt = self._fm_load(l, 6, 4)
    for p in range(2):
        ps = self._proj(wt, p)
        P.copy(qt[:, p, :], ps)
        P.tt(qd[:, p, :].rearrange("p (b t) -> p b t", b=NB), ps.rearrange("p (b t) -> p b t", b=NB),
             c(("qdecay", p)).unsqueeze(1).to_broadcast([128, NB, 128]), ALU.mult)
    for p in range(2):
        P.copy(kt[:, p, :], self._proj(wt, 2 + p))
    wt = self._fm_load(l, 10, 2)
    for p in range(2):
        P.act(sg[:, p, :], self._proj(wt, p), AF.Silu)
    yield
    o_alls = [P.psl(2), P.psl(3)]
    for i in range(NB):
        bs = slice(i * 128, (i + 1) * 128)
        for p in range(2):
            S_ = self.Sr[:, l * 2 + p, :]
            aT = P.psb(2)
            for hh in range(2):
                rs_ = slice(hh * 64, (hh + 1) * 64)
                P.mm(aT[:, hh * 512:hh * 512 + 128], kt[rs_, p, bs], qt[rs_, p, bs])
            att = self.r_att[:, (i * 2 + p) % 2, :]
            P.tt(att.rearrange("p (a n) -> p a n", a=2), aT.rearrange("p (a n) -> p a n", a=2)[:, :, 0:128],
                 c(("decayT", p)).rearrange("p (a n) -> p a n", a=2), ALU.mult)
            yield
            vp = self.m_pad[:, i, :].rearrange("p (a s n) -> p a s n", a=2, s=2)
            P.mm(o_alls[p][:, bs], vp[:, p, 0, :], att[:, 0:128], start=True, stop=False)
            P.mm(o_alls[p][:, bs], vp[:, p, 1, :], att[:, 128:256], start=False, stop=False)
            P.mm(o_alls[p][:, bs], S_, qd[:, p, bs], start=False, stop=True)
            dS = P.psb()
            P.mm(dS[:, 0:128], self.m_tok[:, i, p * 128:(p + 1) * 128], self.m_vtok[:, i, p * 128:(p + 1) * 128])
            P.tt(self.m_tmp128, dS[:, 0:128], c("bdmask"), ALU.mult)
            P.stt(S_, S_, c("g128")[:, p:p + 1], self.m_tmp128, ALU.mult, ALU.add)
            yield
        self._gates_some(l, 1)
    for p in range(2):
        o_ps = o_alls[p]
        P.act(a_, o_ps, AF.Square) if False else P.copy(a_, o_ps)
        obf = kt[:, p, :]
        P.copy(obf, o_ps)
        mean = P.psb()
        P.mm(mean, self.blk64b, obf)
        P.tt(b_, a_, mean, ALU.subtract)
        yield
        sq = qt[:, p, :]
        P.act(sq, b_, AF.Square)
        var = P.psb()
        P.mm(var, self.blk64b, sq)
        self._rs(d_, var)
        yield
        P.tt(c_, b_, d_, ALU.mult)
        P.ts(c_, c_, self.pc(("gn_w", l), p), self.pc(("gn_b", l), p), ALU.mult, ALU.add)
        P.tt(yT[:, p, :], c_, sg[:, p, :], ALU.mult)
        yield


def _swa(self, l, g):
    P = self.P
    c = self.c
    dc = self.dcols[:, l, :]
    yT = self.yT[2]
    a_, b_, c_, d_ = self.r_a, self.r_b, self.r_c, self.r_d
    qh = self.r_qt
    khat = self.khat[:, l, :]
    vp = self.vpad
    wt = self._fm_load(l, 12, 3)
    raws = [a_, b_, c_]
    for k_ in range(3):
        P.copy(raws[k_], self._proj(wt, k_))
    yield
    for which in range(3):
        raw = raws[which]
        wcol = self.pc(("qn_w", l)) if which < 2 else self.pc(("kn_w", l))
        dst = qh[:, which, :] if which < 2 else khat[:, 128:128 + G]
        sq = self.r_kt[:, which % 2, :]
        P.act(sq, raw, AF.Square)
        ms = P.psb()
        P.mm(ms, self.blk64b, sq)
        self._rs(d_, ms)
        P.stt(dst, raw, wcol, d_, ALU.mult, ALU.mult)
        yield
    n_att = 0
    for j in range(2):
        o_all = P.psl(2)
        den = P.psl(3)
        for i in range(NB):
            bs = slice(i * 128, (i + 1) * 128)
            first = (g == 0 and i == 0)
            for hh in range(2):
                h = 2 * j + hh
                kv = j
                rs_ = slice(kv * 64, (kv + 1) * 64)
                ST = P.psb()
                for s_ in range(2):
                    kcols = slice(i * 128 + s_ * 128, i * 128 + s_ * 128 + 128)
                    P.mm(ST[:, s_ * 128:(s_ + 1) * 128], khat[rs_, kcols], qh[rs_, h % 2, bs], start=True, stop=False)
                    bb = self.biasb[:, 8, :] if (first and s_ == 0) else self.biasb[:, h * 2 + s_, :]
                    P.mm(ST[:, s_ * 128:(s_ + 1) * 128], self.identb, bb, start=False, stop=True)
                PT = self.r_att[:, n_att % 2, :]
                n_att += 1
                P.act(PT, ST[:, 0:256], AF.Exp, scale=0.125)
                yield
                for s_ in range(2):
                    vv = vp[:, i + s_, :].rearrange("p (v n) -> p v n", v=4)[:, 2 * kv + hh, :]
                    st_ = (hh == 0 and s_ == 0)
                    sp_ = (hh == 1 and s_ == 1)
                    P.mm(o_all[:, bs], vv, PT[:, s_ * 128:(s_ + 1) * 128], start=st_, stop=sp_)
                    P.mm(den[:, bs], self.onespad[:, hh, :], PT[:, s_ * 128:(s_ + 1) * 128], start=st_, stop=sp_)
            self._gates_some(l, 1)
            yield
        P.ts(a_, den, dc[:, 4 + j:5 + j], None, ALU.add)
        P.op("dve", lambda e, a_=a_, b_=b_: e.reciprocal(b_, a_), [b_], [a_])
        P.tt(yT[:, j, :], o_all, b_, ALU.mult)
        yield
    P.copy(khat[:, 0:128], khat[:, G:G + 128], eng="pool")
    P.copy(self.vcar[:, l, :], vp[:, NB, :], eng="pool")
    yield


def _rglru(self, l, g, j, temps, lgraw, xcb):
    P = self.P
    dc = self.dcols[:, l, :]
    yT = self.yT[3]
    a_, b_, c_, d_, e_ = temps
    wt = self._fm_load(l, 15 + j, 1)
    P.copy(self.lxh[:, j, 0:3], self.lxc[:, l * 2 + j, 0:3], eng="pool")
    P.copy(self.lxh[:, j, 3:3 + G], self._proj(wt, 0))
    wt = self._fm_load(l, 17 + j, 1)
    P.copy(lgraw, self._proj(wt, 0))
    yield
    lx = self.lxh[:, j, :]
    cw = self.pc(("conv_w", l), j * 4, 4)
    xc = a_
    P.ts(xc, lx[:, 0:G], cw[:, 0:1], self.pc(("conv_b", l), j), ALU.mult, ALU.add)
    for jt in range(1, 4):
        P.stt(xc, lx[:, jt:jt + G], cw[:, jt:jt + 1], xc, ALU.mult, ALU.add)
    P.copy(xcb, xc, eng="pool")
    yield
    pr = P.psb()
    pi_ = P.psb()
    P.mm(pr, self.lruw[:, l * 4 + j, :], xcb)
    P.mm(pi_, self.lruw[:, l * 4 + 2 + j, :], xcb)
    P.act(b_, pr, AF.Sigmoid, bias=self.pc(("lru_ba", l), j))
    P.act(c_, pi_, AF.Sigmoid, bias=self.pc(("lru_bx", l), j))
    xg = lgraw
    P.act(d_, xg, AF.Square)
    P.ts(d_, d_, 0.044715, 1.0, ALU.mult, ALU.add)
    P.tt(d_, d_, xg, ALU.mult)
    P.act(d_, d_, AF.Tanh, scale=0.7978845608028654)
    yield
    P.stt(xg, d_, 1.0, xg, ALU.add, ALU.mult)
    P.tt(c_, c_, xc, ALU.mult)
    P.act(d_, b_, AF.Exp, scale=dc[:, 6 + j:7 + j])
    P.act(e_, b_, AF.Exp, scale=dc[:, 8 + j:9 + j])
    P.act(e_, e_, AF.Ln, scale=-1.0, bias=1.0)
    P.act(e_, e_, AF.Exp, scale=0.5)
    yield
    P.tt(c_, c_, e_, ALU.mult)
    hcol = self.hst[:, l, j:j + 1]
    P.scan(b_, d_, c_, hcol, ALU.mult, ALU.add)
    P.copy(hcol, b_[:, G - 1:G], eng="pool")
    yield
    P.stt(yT[:, j, :], b_, 0.5, lgraw, ALU.mult, ALU.mult)
    P.copy(self.lxc[:, l * 2 + j, 0:3], lx[:, G:G + 3], eng="pool")
    self._gates_some(l, 2)
    yield


def _merge(self, l, g):
    P = self.P
    wbv = self.wbf[("wbr", l)].rearrange("(a p) n -> p a n", p=128)
    wov = self.wbf[("wout", l)].rearrange("(c p) n -> p c n", p=128)
    wts = []
    for hf in range(2):
        wt = self.wslot().rearrange("p (a n) -> p a n", a=4)
        self.wload(wt, wbv[:, hf * 4:(hf + 1) * 4, :])
        wts.append(wt)
    wo = self.wslot().rearrange("p (c n) -> p c n", c=NCH)
    self.wload(wo, wov[:, :, 0:512])
    pss = [P.psl(k_) for k_ in range(4)]
    accs = [[self.m_a, self.m_b, self.m_c, self.m_d], [self.r_a, self.r_b, self.r_c, self.r_d],
            [self.m_q[:, 0, :], self.m_q[:, 1, :], self.m_z[:, 0, :], self.m_z[:, 1, :]]]

    def stage1(cch):
        acc = accs[cch % 3]
        for n in range(4):
            gt = self.gatesb[:, n * 8 + cch, :]
            P.act(gt, gt, AF.Sigmoid, bias=self.pc(("gate_bias", l), n * 8 + cch))
        for n in range(4):
            wt = wts[n // 2]
            ps = P.psb()
            for kc in range(2):
                P.mm(ps, wt[:, (n % 2) * 2 + kc, cch * 128:(cch + 1) * 128], self.yT[n][:, kc, :], start=(kc == 0), stop=(kc == 1))
            P.tt(acc[n], ps, self.gatesb[:, n * 8 + cch, :], ALU.mult)

    def stage2(cch):
        acc = accs[cch % 3]
        P.tt(acc[0], acc[0], acc[1], ALU.add, eng="pool")
        P.tt(acc[2], acc[2], acc[3], ALU.add, eng="pool")
        P.tt(self.merged[:, cch, :], acc[0], acc[2], ALU.add, eng="pool")

    stage1(0)
    stage1(1)
    for cch in range(NCH):
        stage2(cch)
        if cch + 2 < NCH:
            stage1(cch + 2)
        for m in range(4):
            P.mm(pss[m], wo[:, cch, m * 128:(m + 1) * 128], self.merged[:, cch, :], start=(cch == 0), stop=(cch == NCH - 1))
    for m in range(4):
        P.tt(self.xT[:, m, :], pss[m], self.xT[:, m, :], ALU.add)
    wo = self.wslot().rearrange("p (c n) -> p c n", c=NCH)
    self.wload(wo, wov[:, :, 512:1024])
    for m in range(4):
        ps = P.psb()
        for cc in range(NCH):
            P.mm(ps, wo[:, cc, m * 128:(m + 1) * 128], self.merged[:, cc, :], start=(cc == 0), stop=(cc == NCH - 1))
        P.tt(self.xT[:, 4 + m, :], ps, self.xT[:, 4 + m, :], ALU.add)


Builder.mixer_setup = _mixer_setup
Builder._gates_some = _gates_some
Builder._fm_load = _fm_load
Builder._proj = _proj
Builder.mixer = _mixer
Builder._rs = _rs
Builder.hgrn = _hgrn
Builder.retention = _retention
Builder.swa = _swa
Builder.rglru = _rglru
Builder.merge = _merge


def host_consts(inp):
    ct, cb = const_table()
    lw = np.zeros((NL, 4, 128, 128), np.float32)
    for l in range(NL):
        for j in range(2):
            for k, nm in ((0, "lru_wa"), (2, "lru_wx")):
                w = np.asarray(inp[nm][l])
                lw[l, k + j, 0:64, 0:64] = w[2 * j]
                lw[l, k + j, 64:128, 64:128] = w[2 * j + 1]
    return {"ctab": ct, "cbf": cb, "lruw": lw}


def kernel(**inputs):
    x = np.asarray(inputs["x"])
    Bn, T, _ = x.shape
    b = Builder(T)
    nc = b.build()
    consts = host_consts(inputs)
    in_maps = []
    for i in range(Bn):
        m = host_inputs(inputs, i)
        m.update(consts)
        in_maps.append(m)
    res = run_bass_kernel_spmd(nc, in_maps, core_ids=list(range(Bn)))
    out = np.stack([np.asarray(r["outT"]).T for r in res.results], axis=0)
    return np.ascontiguousarray(out.astype(np.float32))
```
